# Optimizing a Trainium2 kernel written in Bass

```python
import jax, jax.numpy as jnp
from jax import lax
import numpy as np

D_MODEL = 1024
BATCH = 8
SEQ = 8192
DEPTH = 1

CHUNK = 64
N_HEADS = 4
HEAD_DIM = D_MODEL // 8
D_MLSTM = N_HEADS * HEAD_DIM
CONV_WIDTH = 4
POOL_WINDOWS = (2, 4, 8, 16)
N_POOL_GROUPS = len(POOL_WINDOWS)
POOL_GROUP_DIM = D_MODEL // 8
D_POOL = N_POOL_GROUPS * POOL_GROUP_DIM
D_FF = 256 * ((8 * D_MODEL // 3 + 255) // 256)
FFN_RES_WEIGHT = 0.5
NORM_EPS = 1e-6
FORGET_BIAS = 3.0
IN_SIZES = (2 * D_MLSTM, D_MLSTM, D_MLSTM, D_POOL, D_MODEL, D_MODEL, N_HEADS, N_HEADS)
D_IN = sum(IN_SIZES)

kernel_name = "hybrid_mlstm_pool_macaron"


def rms_norm(x, g):
    xf = x.astype(jnp.float32)
    y = xf * lax.rsqrt(jnp.mean(xf * xf, axis=-1, keepdims=True) + NORM_EPS)
    return (y * g.astype(jnp.float32)).astype(x.dtype)


def swiglu_ffn(x, w_gu, w_down):
    gate, up = jnp.split(x @ w_gu, 2, axis=-1)
    return (jax.nn.silu(gate) * up) @ w_down


def causal_depthwise_conv(x, w):
    k = w.shape[0]
    return lax.conv_general_dilated(
        x, w[:, None, :].astype(x.dtype), window_strides=(1,), padding=[(k - 1, 0)],
        dimension_numbers=("NWC", "WIO", "NWC"), feature_group_count=x.shape[-1])


def mlstm_chunkwise(q, k, v, i_pre, f_pre):
    b_sz, s_len, n_h, d_h = q.shape
    n_c = s_len // CHUNK
    f32 = jnp.float32

    def to_chunks(t):
        t = t.astype(f32).reshape((b_sz, n_c, CHUNK, n_h) + t.shape[3:])
        return jnp.moveaxis(t, (1, 3), (0, 2))

    qc = to_chunks(q)
    kc = to_chunks(k) * (d_h ** -0.5)
    vc = to_chunks(v)
    ic = to_chunks(i_pre)
    fc = to_chunks(jax.nn.log_sigmoid(f_pre.astype(f32)))
    causal = jnp.tril(jnp.ones((CHUNK, CHUNK), dtype=bool))

    def step(carry, xs):
        c_st, n_st, m_st = carry
        q_, k_, v_, i_, lf_ = xs
        b = jnp.cumsum(lf_, axis=-1)
        a_inter = b + m_st[..., None]
        d_log = jnp.where(causal, b[..., :, None] - b[..., None, :] + i_[..., None, :], -jnp.inf)
        m_t = jnp.maximum(a_inter, jnp.max(d_log, axis=-1))
        w_inter = jnp.exp(a_inter - m_t)
        s_qk = jnp.einsum("bhtd,bhsd->bhts", q_, k_) * jnp.exp(d_log - m_t[..., None])
        num = (w_inter[..., None] * jnp.einsum("bhvd,bhtd->bhtv", c_st, q_)
               + jnp.einsum("bhts,bhsv->bhtv", s_qk, v_))
        den = w_inter * jnp.einsum("bhd,bhtd->bht", n_st, q_) + jnp.sum(s_qk, axis=-1)
        h = num / jnp.maximum(jnp.abs(den), jnp.exp(-m_t))[..., None]
        g_s = b[..., -1:] - b + i_
        m_new = jnp.maximum(b[..., -1] + m_st, jnp.max(g_s, axis=-1))
        decay = jnp.exp(b[..., -1] + m_st - m_new)
        w_s = jnp.exp(g_s - m_new[..., None])
        c_new = decay[..., None, None] * c_st + jnp.einsum("bhs,bhsv,bhsd->bhvd", w_s, v_, k_)
        n_new = decay[..., None] * n_st + jnp.einsum("bhs,bhsd->bhd", w_s, k_)
        return (c_new, n_new, m_new), h

    init = (jnp.zeros((b_sz, n_h, d_h, d_h), f32), jnp.zeros((b_sz, n_h, d_h), f32),
            jnp.zeros((b_sz, n_h), f32))
    _, h = lax.scan(step, init, (qc, kc, vc, ic, fc))
    return jnp.moveaxis(h, (0, 2), (1, 3)).reshape(b_sz, s_len, n_h * d_h)


def multiscale_pool(u):
    b_sz, s_len, _ = u.shape
    uf = u.astype(jnp.float32).reshape(b_sz, s_len, N_POOL_GROUPS, POOL_GROUP_DIM)
    csum = jnp.pad(jnp.cumsum(uf, axis=1), ((0, 0), (1, 0), (0, 0), (0, 0)))
    pos = jnp.arange(1, s_len + 1, dtype=jnp.float32)
    groups = []
    for gi, win in enumerate(POOL_WINDOWS):
        upper = csum[:, 1:, gi]
        lower = jnp.pad(csum[:, :s_len + 1 - win, gi], ((0, 0), (win - 1, 0), (0, 0)))
        count = jnp.minimum(pos, float(win))[None, :, None]
        groups.append((upper - lower) / count - uf[:, :, gi])
    return jnp.stack(groups, axis=2)


def hybrid_mixer(h, w_in, b_in, conv_qk, head_norm_g, p_a, w_pool, pool_scale, p_b, w_out):
    b_sz, s_len, _ = h.shape
    z = h @ w_in + b_in
    parts = []
    start = 0
    for size in IN_SIZES:
        parts.append(z[..., start:start + size])
        start += size
    qk, v, o, u, gate_a, gate_b, i_pre, f_pre = parts
    qk = jax.nn.silu(causal_depthwise_conv(qk, conv_qk))
    q, k = jnp.split(qk, 2, axis=-1)
    hs = (b_sz, s_len, N_HEADS, HEAD_DIM)
    y_a = mlstm_chunkwise(q.reshape(hs), k.reshape(hs), v.reshape(hs), i_pre, f_pre)
    y_a = y_a.reshape(hs)
    y_a = (y_a * lax.rsqrt(jnp.mean(y_a * y_a, axis=-1, keepdims=True) + NORM_EPS)).reshape(b_sz, s_len, D_MLSTM)
    y_a = (y_a * head_norm_g.astype(jnp.float32) * jax.nn.sigmoid(o.astype(jnp.float32))).astype(h.dtype)
    pooled = multiscale_pool(u)
    y_b = jnp.einsum("bsgc,gcd->bsgd", pooled.astype(h.dtype), w_pool).reshape(b_sz, s_len, D_POOL) * pool_scale
    merged = jax.nn.sigmoid(gate_a) * (y_a @ p_a) + jax.nn.sigmoid(gate_b) * (y_b @ p_b)
    return merged @ w_out


def setup_inputs(seed: int = 0) -> dict:
    key = jax.random.key(seed)
    ks = jax.random.split(key, 20)
    L = DEPTH
    nrm = lambda k, shape, fan: jax.random.normal(k, shape, jnp.float32) * (fan ** -0.5)
    gain = lambda k, shape: 1.0 + 0.05 * jax.random.normal(k, shape, jnp.float32)
    f_offset = jnp.zeros((D_IN,), jnp.float32).at[D_IN - N_HEADS:].set(FORGET_BIAS)
    b_in = 0.02 * jax.random.normal(ks[5], (L, D_IN), jnp.float32) + f_offset
    return {
        "x": jax.random.normal(ks[0], (BATCH, SEQ, D_MODEL), jnp.float32),
        "ffn1_norm_g": gain(ks[1], (L, D_MODEL)),
        "ffn1_w_gu": nrm(ks[2], (L, D_MODEL, 2 * D_FF), D_MODEL),
        "ffn1_w_down": nrm(ks[3], (L, D_FF, D_MODEL), D_FF),
        "mix_norm_g": gain(ks[4], (L, D_MODEL)),
        "w_in": nrm(ks[6], (L, D_MODEL, D_IN), D_MODEL),
        "b_in": b_in,
        "conv_qk": nrm(ks[7], (L, CONV_WIDTH, 2 * D_MLSTM), CONV_WIDTH),
        "head_norm_g": gain(ks[8], (L, D_MLSTM)),
        "p_a": nrm(ks[9], (L, D_MLSTM, D_MODEL), D_MLSTM),
        "w_pool": nrm(ks[10], (L, N_POOL_GROUPS, POOL_GROUP_DIM, POOL_GROUP_DIM), POOL_GROUP_DIM),
        "pool_scale": gain(ks[11], (L, D_POOL)),
        "p_b": nrm(ks[12], (L, D_POOL, D_MODEL), D_POOL),
        "w_out": nrm(ks[13], (L, D_MODEL, D_MODEL), D_MODEL),
        "ffn2_norm_g": gain(ks[14], (L, D_MODEL)),
        "ffn2_w_gu": nrm(ks[15], (L, D_MODEL, 2 * D_FF), D_MODEL),
        "ffn2_w_down": nrm(ks[16], (L, D_FF, D_MODEL), D_FF),
        "final_norm_g": gain(ks[17], (D_MODEL,)),
    }


def reference(x, ffn1_norm_g, ffn1_w_gu, ffn1_w_down, mix_norm_g, w_in, b_in, conv_qk,
              head_norm_g, p_a, w_pool, pool_scale, p_b, w_out, ffn2_norm_g, ffn2_w_gu,
              ffn2_w_down, final_norm_g):
    for l in range(DEPTH):
        x = x + FFN_RES_WEIGHT * swiglu_ffn(rms_norm(x, ffn1_norm_g[l]), ffn1_w_gu[l], ffn1_w_down[l])
        x = x + hybrid_mixer(rms_norm(x, mix_norm_g[l]), w_in[l], b_in[l], conv_qk[l], head_norm_g[l],
                             p_a[l], w_pool[l], pool_scale[l], p_b[l], w_out[l])
        x = x + FFN_RES_WEIGHT * swiglu_ffn(rms_norm(x, ffn2_norm_g[l]), ffn2_w_gu[l], ffn2_w_down[l])
    return rms_norm(x, final_norm_g)
```

```python
import numpy as np
from contextlib import ExitStack

import concourse.bass as bass
import concourse.mybir as mybir
from concourse.bass_utils import run_bass_kernel_spmd

F32 = mybir.dt.float32
BF16 = mybir.dt.bfloat16
AF = mybir.ActivationFunctionType
ALU = mybir.AluOpType

D = 1024
SEQ = 8192
NB = 8
T = 512
NSUB = 4
KC = 8
DFF = 2816
NFC = 22
H = 4
EPS = 1e-6
SCALE = 128.0 ** -0.5
NPIECE = 51
NSLOT = 5
NCONV = 8

G1, GM, G2, BQK, BO, BGA, BGB, HNG, PSC, CONV = 0, 8, 16, 24, 32, 36, 44, 52, 56, 60
NPV = 92
C_ID, C_TRI, C_ONE, C_BAND = 0, 128, 256, 384
NCST = 384 + 12 * 128


def _esz(dt):
    return 2 if dt == BF16 else 4


class Chan:
    def __init__(self, name, is_dma, eng=None):
        self.name = name
        self.is_dma = is_dma
        self.eng = eng
        self.ops = []
        self.sem = None


class Op:
    __slots__ = ("eng", "chan", "fn", "deps", "needs_inc", "idx", "value")


class Rec:
    __slots__ = ("lo", "hi", "writers", "readers", "ov")


class Prog:
    ENGS = ("pe", "act", "dve", "pool", "sp")

    def __init__(self):
        self.eng_ops = {e: [] for e in self.ENGS}
        self.dma_chans = []
        self.all_eng_chans = []
        self.epoch = -1
        self.new_epoch()
        self.state = {}
        self._bank = 0

    def new_epoch(self):
        self.epoch += 1
        self.eng_chan = {e: Chan(f"{e}{self.epoch}", False, e) for e in ("pe", "act", "dve", "pool")}
        self.all_eng_chans += list(self.eng_chan.values())

    def dma_chan(self, name):
        c = Chan(name, True)
        self.dma_chans.append(c)
        return c

    def _key(self, a):
        if isinstance(a, tuple):
            return a
        dims = a.ap
        pstep = dims[0][0]
        start = a.offset % pstep if pstep > 0 else a.offset
        lo = hi = start
        for step, cnt in dims[1:]:
            if step >= 0:
                hi += step * (cnt - 1)
            else:
                lo += step * (cnt - 1)
        e = _esz(a.dtype)
        lo, hi = lo * e, (hi + 1) * e
        if a.tensor.name == "ps":
            lo = (lo // 2048) * 2048
            hi = ((hi + 2047) // 2048) * 2048
        return (a.tensor.name, lo, hi)

    def _rec(self, key):
        name, lo, hi = key
        recs = self.state.setdefault(name, {})
        r = recs.get((lo, hi))
        if r is None:
            r = Rec()
            r.lo, r.hi, r.writers, r.readers, r.ov = lo, hi, {}, {}, []
            for o in recs.values():
                if o.lo < hi and lo < o.hi:
                    o.ov.append(r)
                    r.ov.append(o)
            r.ov.append(r)
            recs[(lo, hi)] = r
        return r

    def op(self, eng, fn, reads=(), writes=(), chan=None):
        o = Op()
        o.eng, o.fn, o.needs_inc, o.value = eng, fn, False, None
        o.chan = chan if chan is not None else self.eng_chan[eng]
        o.idx = len(o.chan.ops)
        deps = {}

        def add(d):
            cur = deps.get(d.chan)
            if cur is None or d.idx > cur.idx:
                deps[d.chan] = d

        if o.chan.is_dma and o.idx > 0:
            add(o.chan.ops[o.idx - 1])
        rkeys = [self._key(a) for a in reads]
        wkeys = [self._key(a) for a in writes]
        psk = [k for k in rkeys + wkeys if k[0] == "ps"]
        rkeys = [k for k in rkeys if k[0] != "ps"]
        wkeys = [k for k in wkeys if k[0] != "ps"]
        for k in psk:
            for b in range(k[1] // 2048, k[2] // 2048):
                kb = ("ps", b * 2048, (b + 1) * 2048)
                if kb not in wkeys:
                    wkeys.append(kb)
        rrecs = [self._rec(k) for k in rkeys]
        wrecs = [self._rec(k) for k in wkeys]
        for r in rrecs:
            for x in r.ov:
                for w in x.writers.values():
                    add(w)
        for r in wrecs:
            for x in r.ov:
                for w in x.writers.values():
                    add(w)
                for w in x.readers.values():
                    add(w)
        for r in rrecs:
            r.readers[o.chan] = o
        for r in wrecs:
            for x in r.ov:
                if x is r:
                    x.writers = {o.chan: o}
                    x.readers = {}
                else:
                    x.writers[o.chan] = o
        if eng == "pe":
            for ch in [c for c in deps if c.eng == "pe"]:
                del deps[ch]
        o.deps = deps
        o.chan.ops.append(o)
        self.eng_ops[eng].append(o)
        return o

    def bank(self):
        b = self._bank
        self._bank = (b + 1) % 7
        return b

    def bank2(self):
        b = self._bank
        while b % 2 or b >= 6:
            b = (b + 1) % 7
        self._bank = (b + 2) % 7
        return b

    def finalize(self, nc, es):
        for ops in self.eng_ops.values():
            for o in ops:
                for d in o.deps.values():
                    d.needs_inc = True
        for c in [c for c in self.all_eng_chans if c.ops] + self.dma_chans:
            c.sem = es.enter_context(nc.semaphore(c.name))
            n = 0
            for o in c.ops:
                if c.is_dma:
                    n += 16
                    o.needs_inc = True
                elif o.needs_inc:
                    n += 1
                o.value = n
        block = es.enter_context(nc.Block())
        names = {"pe": "tensor", "act": "scalar", "dve": "vector", "pool": "gpsimd", "sp": "sync"}

        def make(eng):
            ops = self.eng_ops[eng]

            def body(e):
                waited = {}
                last_dma = {}
                for o in ops:
                    for ch, d in o.deps.items():
                        if waited.get(ch, 0) < d.value:
                            e.wait_ge(ch.sem, d.value)
                            waited[ch] = d.value
                    ins = o.fn(e)
                    if o.needs_inc:
                        ins.then_inc(o.chan.sem, 16 if o.chan.is_dma else 1)
                    if o.chan.is_dma:
                        last_dma[o.chan] = o.value
                for ch, v in last_dma.items():
                    if waited.get(ch, 0) < v:
                        e.wait_ge(ch.sem, v)
            return body

        for eng in self.ENGS:
            if self.eng_ops[eng]:
                getattr(block, names[eng])(make(eng))


def build_nc(nt, first_tile_global=True, dbg=None, stop=None):
    nc = bass.Bass("TRN2", target_bir_lowering=False)
    ntok = nt * T
    x_d = nc.dram_tensor("x", [ntok, D], F32, kind="ExternalInput").ap()
    wsrc_d = nc.dram_tensor("wsrc", [NPIECE, 128, 4096], F32, kind="ExternalInput").ap()
    pv_d = nc.dram_tensor("pv", [128, NPV], F32, kind="ExternalInput").ap()
    rowp_d = nc.dram_tensor("rowp", [1, 8 + 512 + 1024], F32, kind="ExternalInput").ap()
    wsm_d = nc.dram_tensor("wsm", [128, 576], F32, kind="ExternalInput").ap()
    cst_d = nc.dram_tensor("cst", [128, NCST], F32, kind="ExternalInput").ap()
    out_d = nc.dram_tensor("out", [ntok, D], F32, kind="ExternalOutput").ap()
    scr_d = nc.dram_tensor("scr", [NPIECE, 128, 4096], BF16).ap()
    dbg_outs = {}

    P = Prog()
    es = ExitStack()
    with es:
        def sb(name, shape, dt):
            return es.enter_context(nc.sbuf_tensor(name, shape, dt))

        stage_in = sb("stage_in", [128, NSUB, D], F32)
        stage_out = sb("stage_out", [128, 4, D], F32)
        xT = sb("xT", [128, KC, T], F32)
        xn = sb("xn", [128, KC, T], BF16)
        rs = sb("rs", [128, 2, T], F32)
        hid = sb("hid", [128, NFC, T], BF16)
        tmpf = sb("tmpf", [128, 2, T], F32)
        zqk = sb("zqk", [128, KC, 516], BF16)
        qkT = sb("qkT", [128, KC, T], BF16)
        ktm = sb("ktm", [128, NSUB, T], BF16)
        vaug = sb("vaug", [128, NSUB, H, 130], BF16)
        U = sb("U", [128, 5, T], BF16)
        poolT = sb("poolT", [128, 4, T], BF16)
        ybT = sb("ybT", [128, 4, T], BF16)
        spT = sb("spT", [128, 2, T], BF16)
        ytm = sb("ytm", [128, 2, T], BF16)
        yaT = sb("yaT", [128, H, T], BF16)
        Cf = sb("Cf", [128, H, 130], F32)
        Cbf = sb("Cbf", [128, H, 130], BF16)
        zif = sb("zif", [128, NSUB, 8], F32)
        ef = sb("ef", [128, NSUB, 4], F32)
        nlf = sb("nlf", [128, NSUB, 4], F32)
        eb = sb("eb", [128, NSUB, 4], F32)
        eB = sb("eB", [128, NSUB, 4], F32)
        inv2 = sb("inv2", [128, NSUB, 4], F32)
        tmpi = sb("tmpi", [128, NSUB, 4], F32)
        wgt = sb("wgt", [128, NSUB, 4], F32)
        sc = sb("sc", [128, 10, 4], F32)
        ss = sb("ss", [128, NSUB, 4], F32)
        frs = sb("frs", [128, 2, 4], F32)
        junk = sb("junk", [128, 128], BF16)
        dmy = sb("dmy", [128, 4], F32)
        cbf = sb("cbf", [128, NCST], BF16)
        c32 = sb("c32", [128, 384], F32)
        diag = sb("diag", [128, 32, 128], BF16)
        wsm = sb("wsm_s", [128, 576], BF16)
        pv = sb("pv_s", [128, NPV], F32)
        bif = sb("bif", [128, 8], F32)
        bv = sb("bv", [1, 512], BF16)
        gfin = sb("gfin", [128, D], F32)
        wslot = [sb(f"wslot{i}", [128, 4096], BF16) for i in range(NSLOT)]
        ps = es.enter_context(nc.psum_tensor("ps", [128, 4096], F32))
        psb = ps[:, :].bitcast(BF16)

        def PS(b, lo=0, hi=512):
            return ps[:, b * 512 + lo: b * 512 + hi]

        def PSB(b, lo, hi):
            return psb[:, b * 1024 + lo: b * 1024 + hi]

        ident32 = c32[:, C_ID:C_ID + 128]
        tri32 = c32[:, C_TRI:C_TRI + 128]
        ones32 = c32[:, C_ONE:C_ONE + 128]
        identb = cbf[:, C_ID:C_ID + 128]
        trib = cbf[:, C_TRI:C_TRI + 128]
        onesb = cbf[:, C_ONE:C_ONE + 128]

        def band(kind, g):
            o = C_BAND + (kind * 4 + g) * 128
            return cbf[:, o:o + 128]

        def pcol(c, n=1):
            return pv[:, c:c + n]

        def mm(out, lhsT, rhs, start, stop):
            P.op("pe", lambda e: e.matmul(out, lhsT, rhs, start=start, stop=stop),
                 reads=[lhsT, rhs], writes=[out])

        def tr(out, in_, ident):
            P.op("pe", lambda e: e.transpose(out, in_, ident), reads=[in_, ident], writes=[out])

        def act(out, in_, func, bias=None, scale=None, accum=None):
            kw = {}
            rd = [in_]
            wr = [out]
            if bias is not None:
                kw["bias"] = bias
                if not isinstance(bias, float):
                    rd.append(bias)
            if scale is not None:
                kw["scale"] = scale
                if not isinstance(scale, float):
                    rd.append(scale)
            if accum is not None:
                kw["accum_out"] = accum
                wr.append(accum)
            P.op("act", lambda e: e.activation(out, in_, func, **kw), reads=rd, writes=wr)

        def tt(out, in0, in1, op, eng="dve"):
            P.op(eng, lambda e: e.tensor_tensor(out, in0, in1, op), reads=[in0, in1], writes=[out])

        def stt(out, in0, scalar, in1, op0, op1):
            rd = [in0, in1] + ([] if isinstance(scalar, float) else [scalar])
            P.op("dve", lambda e: e.scalar_tensor_tensor(out, in0, scalar, in1, op0, op1),
                 reads=rd, writes=[out])

        def ts(out, in0, s1, op0, s2=None, op1=None):
            rd = [in0] + ([] if isinstance(s1, float) else [s1])
            if op1 is None:
                P.op("dve", lambda e: e.tensor_single_scalar(out, in0, s1, op0), reads=rd, writes=[out])
            else:
                P.op("dve", lambda e: e.tensor_scalar(out, in0, s1, s2, op0, op1), reads=rd, writes=[out])

        def cp(out, in_, eng="dve"):
            if eng == "act":
                P.op("act", lambda e: e.copy(out, in_), reads=[in_], writes=[out])
            else:
                P.op(eng, lambda e: e.tensor_copy(out, in_), reads=[in_], writes=[out])

        def memset(ap, v, eng="dve"):
            P.op(eng, lambda e: e.memset(ap, v), writes=[ap])

        def dma(eng, chan, out, in_, reads=(), writes=()):
            P.op(eng, lambda e: e.dma_start(out=out, in_=in_), reads=list(reads), writes=list(writes), chan=chan)

        def dump(name, ap, shape, dt=F32):
            if dbg is None or name not in dbg:
                return
            d = nc.dram_tensor("dbg_" + name, list(shape), dt, kind="ExternalOutput").ap()
            dbg_outs[name] = d
            dma("pool", P.dma_chan("dbg_" + name), d, ap, reads=[ap])

        ch_xin = P.dma_chan("xin")
        ch_out = [P.dma_chan(f"out{q}") for q in range(4)]
        ch_w = [P.dma_chan(f"w{i}") for i in range(NSLOT)]
        ch_st = [P.dma_chan(f"st{i}") for i in range(NSLOT)]
        ch_wc = [P.dma_chan(f"wc{i}") for i in range(NSLOT)]

        def load_x(i):
            src = x_d[i * T:(i + 1) * T, :].rearrange("(j p) d -> p j d", p=128)
            dma("pool", ch_xin, stage_in[:, :, :], src, writes=[stage_in[:, :, :]])

        for nm, dst, src in (
            ("pv", pv[:, :], pv_d),
            ("c32", c32[:, :], cst_d[:, 0:384]),
            ("cbf", cbf[:, :], cst_d),
            ("wsm", wsm[:, :], wsm_d),
            ("bif", bif[:, :], rowp_d[:, 0:8].partition_broadcast(128)),
            ("bv", bv[:, :], rowp_d[:, 8:520]),
            ("gfin", gfin[:, :], rowp_d[:, 520:1544].partition_broadcast(128)),
        ):
            dma("pool", P.dma_chan("su_" + nm), dst, src, writes=[dst])
        load_x(0)
        memset(dmy[:, :], 1.0)
        memset(Cf[:, :, :], 0.0)
        memset(Cbf[:, :, :], 0.0)
        memset(zqk[:, :, 0:3], 0.0)
        memset(U[:, 0, :], 0.0)
        memset(vaug[:, :, :, :], 0.0)
        for j in range(4):
            for ch in range(KC):
                ts(diag[:, j * 8 + ch, :], identb, pcol(CONV + j * 8 + ch), ALU.mult)

        wstate = {"n": 0}

        converted = set()

        def load_piece(k):
            s = wstate["n"] % NSLOT
            wstate["n"] += 1
            nco = 2816 if (11 <= k < 19 or 43 <= k < 51) else 4096
            if k not in converted:
                converted.add(k)
                dma("pool", ch_wc[s], wslot[s][:, 0:nco], wsrc_d[k][:, 0:nco], writes=[wslot[s][:, :]])
                dma("sp", ch_st[s], scr_d[k][:, 0:nco], wslot[s][:, 0:nco], reads=[wslot[s][:, :]], writes=[("scr", k, k + 1)])
            else:
                dma("sp", ch_w[s], wslot[s][:, 0:nco], scr_d[k][:, 0:nco], reads=[("scr", k, k + 1)], writes=[wslot[s][:, :]])
            return wslot[s]

        def preload_ln():
            act(dmy[:, 2:3], dmy[:, 0:1], AF.Ln)

        NBANK = 7

        sq_pending = []

        def sq_flush():
            while sq_pending:
                k, buf = sq_pending.pop(0)
                mm(PS(NBANK), onesb, buf[:, k, :], k == 0, k == KC - 1)

        def sq_chunk(k, buf):
            act(buf[:, k, :], xT[:, k, :], AF.Square)
            sq_flush()
            sq_pending.append((k, buf))

        def norm_finish(gcol):
            sq_flush()
            act(rs[:, 0, :], PS(NBANK), AF.Ln, bias=EPS, scale=1.0 / D)
            act(rs[:, 1, :], rs[:, 0, :], AF.Exp, scale=-0.5)
            for k in range(KC):
                stt(xn[:, k, :], xT[:, k, :], pcol(gcol + k), rs[:, 1, :], ALU.mult, ALU.mult)

        def ffn(piece0, gcol, after_chunk):
            norm_finish(gcol)
            for p in range(11):
                w = load_piece(piece0 + p)
                for jj in range(2):
                    j = 2 * p + jj
                    bg = P.bank()
                    for k in range(KC):
                        mm(PS(bg), w[:, k * 512 + jj * 128: k * 512 + jj * 128 + 128], xn[:, k, :], k == 0, k == KC - 1)
                    bu = P.bank()
                    for k in range(KC):
                        mm(PS(bu), w[:, k * 512 + 256 + jj * 128: k * 512 + 256 + jj * 128 + 128], xn[:, k, :], k == 0, k == KC - 1)
                    act(tmpf[:, j % 2, :], PS(bg), AF.Silu)
                    tt(hid[:, j, :], tmpf[:, j % 2, :], PS(bu), ALU.mult)
            preload_ln()
            for j in range(KC):
                w = load_piece(piece0 + 11 + j)
                b = P.bank()
                for k in range(NFC):
                    mm(PS(b), w[:, k * 128:(k + 1) * 128], hid[:, k, :], k == 0, k == NFC - 1)
                stt(xT[:, j, :], PS(b), 0.5, xT[:, j, :], ALU.mult, ALU.add)
                after_chunk(j)

        def proj_fm(w, cc, evac, bank=None):
            b = P.bank() if bank is None else bank
            for k in range(KC):
                mm(PS(b), w[:, k * 512 + cc * 128: k * 512 + cc * 128 + 128], xn[:, k, :], k == 0, k == KC - 1)
            evac(b)

        def mixer(i):
            norm_finish(GM)
            bgt = P.bank()
            for j in range(NSUB):
                for k in range(KC):
                    mm(PS(bgt, j * 8, j * 8 + 8), xn[:, k, j * 128:(j + 1) * 128], wsm[:, k * 8:(k + 1) * 8], k == 0, k == KC - 1)
            tt(zif[:, :, :], PS(bgt, 0, 32).rearrange("p (j c) -> p j c", j=NSUB),
               bif[:, :].unsqueeze(1).to_broadcast([128, NSUB, 8]), ALU.add)
            act(ef[:, :, :], zif[:, :, 4:8], AF.Exp, scale=-1.0)
            act(nlf[:, :, :], ef[:, :, :], AF.Ln, bias=1.0)
            bcs = P.bank()
            for j in range(NSUB):
                mm(PS(bcs, j * 4, j * 4 + 4), tri32, nlf[:, j, :], True, True)
            for j in range(NSUB):
                mm(PS(bcs, 16 + j * 4, 16 + j * 4 + 4), ones32, nlf[:, j, :], True, True)
            act(eb[:, :, :], PS(bcs, 0, 16).rearrange("p (j c) -> p j c", j=NSUB), AF.Exp, scale=-1.0)
            act(eB[:, :, :], PS(bcs, 16, 32).rearrange("p (j c) -> p j c", j=NSUB), AF.Exp, scale=-1.0)
            act(inv2[:, :, :], PS(bcs, 0, 16).rearrange("p (j c) -> p j c", j=NSUB), AF.Exp, scale=2.0)
            tt(tmpi[:, :, :], zif[:, :, 0:4], PS(bcs, 0, 16).rearrange("p (j c) -> p j c", j=NSUB), ALU.add)
            act(wgt[:, :, :], tmpi[:, :, :], AF.Exp)
            cp(vaug[:, :, :, 128:129], wgt[:, :, :].unsqueeze(3))
            if stop == "mx_gates":
                return True
            for pc in range(2):
                w = load_piece(19 + pc)
                for cc in range(4):
                    ch = pc * 4 + cc
                    proj_fm(w, cc, lambda b, ch=ch: ts(zqk[:, ch, 3:515], PS(b), pcol(BQK + ch), ALU.add))
            if stop == "mx_qk":
                return True
            for ch in range(KC):
                b = P.bank()
                for j in range(4):
                    mm(PS(b), diag[:, j * 8 + ch, :], zqk[:, ch, j:j + 512], j == 0, j == 3)
                act(qkT[:, ch, :], PS(b), AF.Silu)
            cp(zqk[:, :, 0:3], zqk[:, :, 512:515])
            if stop == "mx_conv":
                return True
            for j in range(NSUB):
                b = P.bank()
                for h in range(H):
                    tr(PSB(b, h * 128, (h + 1) * 128), qkT[:, 4 + h, j * 128:(j + 1) * 128], identb)
                cp(ktm[:, j, :], PSB(b, 0, 512), eng="act")
            if stop == "mx_ktr":
                return True
            w = load_piece(21)
            for j in range(NSUB):
                b = P.bank()
                for k in range(KC):
                    mm(PS(b), xn[:, k, j * 128:(j + 1) * 128], w[:, k * 512:(k + 1) * 512], k == 0, False)
                mm(PS(b), cbf[0:1, C_ONE:C_ONE + 128], bv[0:1, :], False, True)
                tt(vaug[:, j, :, 0:128], PS(b).rearrange("p (h v) -> p h v", h=H),
                   wgt[:, j, :].unsqueeze(2).to_broadcast([128, H, 128]), ALU.mult)
            if stop == "mx_v":
                return True
            w = load_piece(22)
            for cc in range(4):
                proj_fm(w, cc, lambda b, cc=cc: act(hid[:, 16 + cc, :], PS(b), AF.Sigmoid, bias=pcol(BO + cc)))
            if stop == "mx_o":
                return True
            preload_ln()
            if i > 0:
                cp(U[:, 0, :], U[:, 4, :])
            w = load_piece(23)
            for j in range(NSUB):
                b = P.bank()
                for k in range(KC):
                    mm(PS(b), xn[:, k, j * 128:(j + 1) * 128], w[:, k * 512:(k + 1) * 512], k == 0, k == KC - 1)
                cp(U[:, 1 + j, :], PS(b), eng="act")
            for g in range(4):
                b = P.bank()
                for j in range(NSUB):
                    first = first_tile_global and i == 0 and j == 0
                    mm(PS(b, j * 128, (j + 1) * 128), U[:, j, g * 128:(g + 1) * 128], band(1, g), True, False)
                    mm(PS(b, j * 128, (j + 1) * 128), U[:, j + 1, g * 128:(g + 1) * 128], band(2 if first else 0, g), False, True)
                cp(poolT[:, g, :], PS(b))
            for g in range(4):
                b = P.bank()
                mm(PS(b), wsm[:, 64 + g * 128: 64 + (g + 1) * 128], poolT[:, g, :], True, True)
                act(ybT[:, g, :], PS(b), AF.Copy, scale=pcol(PSC + g))
            if stop == "mx_pool":
                return True
            gate_w = {}

            def gate_filler(which, c):
                def run(bank=None):
                    pidx = (24 if which == 0 else 26) + c // 4
                    if pidx not in gate_w:
                        gate_w[pidx] = load_piece(pidx)
                    bcol = (BGA if which == 0 else BGB) + c
                    dst = hid[:, (0 if which == 0 else 8) + c, :]
                    proj_fm(gate_w[pidx], c % 4, lambda b: act(dst, PS(b), AF.Identity, bias=pcol(bcol)), bank=bank)
                return run

            fillers = [gate_filler(wh, c) for wh in range(2) for c in range(KC)]

            def fill():
                if fillers:
                    fillers.pop(0)()

            memset(ss[:, :, :], 0.0)
            small = {"n": 0}

            def small_bank():
                small["n"] += 1
                return 6 + small["n"] % 2

            ACCB = (0, 2)
            CBANK = 4
            S_ = lambda n: sc[:, n, :]

            def st_S(j):
                tok = slice(j * 128, (j + 1) * 128)
                bS = small_bank()
                for h in range(H):
                    mm(PS(bS, h * 128, (h + 1) * 128), qkT[:, 4 + h, tok], qkT[:, h, tok], True, True)
                tt(spT[:, j % 2, :].rearrange("p (h t) -> p h t", h=H), PS(bS).rearrange("p (h t) -> p h t", h=H),
                   trib.unsqueeze(1).to_broadcast([128, H, 128]), ALU.mult)

            def st_A(j):
                tok = slice(j * 128, (j + 1) * 128)
                par = j % 2
                bA = ACCB[par]
                for h in range(H):
                    o_ = ps[:, bA * 512 + h * 256: bA * 512 + h * 256 + 130]
                    mm(o_, spT[:, par, h * 128:(h + 1) * 128], vaug[:, j, h, 0:130], True, False)
                    mm(o_, qkT[:, h, tok], Cbf[:, h, 0:130], False, True)
                acc = ps[:, bA * 512: bA * 512 + 1024].rearrange("p (h c) -> p h c", h=H)
                for h in range(H):
                    act(junk[:, :], acc[:, h, 0:128], AF.Square, accum=ss[:, j, h:h + 1])
                bC = CBANK
                for h in range(H):
                    mm(ps[:, bC * 512 + h * 256: bC * 512 + h * 256 + 130], ktm[:, j, h * 128:(h + 1) * 128], vaug[:, j, h, 0:130], True, True)
                accC = ps[:, bC * 512: bC * 512 + 1024].rearrange("p (h c) -> p h c", h=H)
                tt(Cf[:, :, :], Cf[:, :, :], accC[:, :, 0:130], ALU.add)
                tt(Cf[:, :, :], Cf[:, :, :], eB[:, j, :].unsqueeze(2).to_broadcast([128, H, 130]), ALU.mult)
                cp(Cbf[:, :, :], Cf[:, :, :])

            def st_scal(j):
                par = j % 2
                bA = ACCB[par]
                acc = ps[:, bA * 512: bA * 512 + 1024].rearrange("p (h c) -> p h c", h=H)
                stt(S_(0).unsqueeze(2), acc[:, :, 128:129], SCALE, eb[:, j, :].unsqueeze(2), ALU.mult, ALU.mult)
                tt(S_(1), S_(0), S_(0), ALU.mult)
                ts(S_(2), S_(1), 1.0, ALU.max, EPS / (SCALE * SCALE), ALU.mult)
                tt(S_(3), S_(2), inv2[:, j, :], ALU.mult)
                stt(S_(4), ss[:, j, :], 1.0 / 128.0, S_(3), ALU.mult, ALU.add)
                act(S_(5), S_(4), AF.Ln)
                act(S_(6), S_(5), AF.Exp, scale=-0.5)
                tt(ytm[:, par, :].rearrange("p (h v) -> p h v", h=H), acc[:, :, 0:128],
                   S_(6).unsqueeze(2).to_broadcast([128, H, 128]), ALU.mult)

            def st_T(j):
                tok = slice(j * 128, (j + 1) * 128)
                par = j % 2
                bT = small_bank()
                for h in range(H):
                    tr(PSB(bT, h * 128, (h + 1) * 128), ytm[:, par, h * 128:(h + 1) * 128], identb)
                for h in range(H):
                    stt(yaT[:, h, tok], PSB(bT, h * 128, (h + 1) * 128), pcol(HNG + h), hid[:, 16 + h, tok], ALU.mult, ALU.mult)

            def fillS():
                if fillers:
                    fillers.pop(0)(small_bank())

            st_S(0)
            fillS()
            st_A(0)
            fillS()
            for j in range(NSUB):
                if j + 1 < NSUB:
                    st_S(j + 1)
                    fillS()
                st_scal(j)
                if j + 1 < NSUB:
                    st_A(j + 1)
                    fillS()
                if j >= 1:
                    st_T(j - 1)
                    fillS()
            st_T(NSUB - 1)
            if stop == "mx_lstm":
                return True
            while fillers:
                fillers.pop(0)()
            for c in range(16):
                act(hid[:, c, :], hid[:, c, :], AF.Sigmoid)
            preload_ln()
            wa = load_piece(28)
            wb = load_piece(29)
            for c in range(KC):
                ba = P.bank()
                for k in range(4):
                    mm(PS(ba), wa[:, k * 1024 + c * 128: k * 1024 + c * 128 + 128], yaT[:, k, :], k == 0, k == 3)
                bb = P.bank()
                for k in range(4):
                    mm(PS(bb), wb[:, k * 1024 + c * 128: k * 1024 + c * 128 + 128], ybT[:, k, :], k == 0, k == 3)
                tt(tmpf[:, 0, :], PS(ba), hid[:, c, :], ALU.mult)
                tt(tmpf[:, 1, :], PS(bb), hid[:, 8 + c, :], ALU.mult)
                tt(xn[:, c, :], tmpf[:, 0, :], tmpf[:, 1, :], ALU.add)
            if stop == "mx_merge":
                return True
            for pc in range(2):
                w = load_piece(30 + pc)
                for cc in range(4):
                    c = pc * 4 + cc
                    proj_fm(w, cc, lambda b, c=c: tt(xT[:, c, :], PS(b), xT[:, c, :], ALU.add))
                    sq_chunk(c, qkT)

        for i in range(nt if stop != "setup" else 0):
            if i > 0:
                P.new_epoch()
            for k in range(KC):
                b = (6, 2, 3, 4, 5, 6, 2, 3)[k]
                for j in range(NSUB):
                    tr(PS(b, j * 128, (j + 1) * 128), stage_in[:, j, k * 128:(k + 1) * 128], ident32)
                cp(xT[:, k, :], PS(b), eng=("act" if (k < 4 or k % 2) else "dve"))
                sq_chunk(k, xn)
            if i + 1 < nt:
                load_x(i + 1)
            if i == 0:
                dump("x0", xT[:, :, :], [128, KC, T])
            if stop == "input":
                break
            ffn(0, G1, lambda j: sq_chunk(j, xn))
            if i == 0:
                dump("x1", xT[:, :, :], [128, KC, T])
            if stop == "ffn1":
                break
            if mixer(i):
                break
            if i == 0:
                dump("x2", xT[:, :, :], [128, KC, T])
                dump("qkT", qkT[:, :, :], [128, KC, T], BF16)
                dump("yaT", yaT[:, :, :], [128, H, T], BF16)
                dump("ybT", ybT[:, :, :], [128, 4, T], BF16)
                dump("vaug", vaug[:, :, :, :], [128, NSUB, H, 130], BF16)
                dump("eb", eb[:, :, :], [128, NSUB, 4])
                dump("wgt", wgt[:, :, :], [128, NSUB, 4])
                dump("Cf", Cf[:, :, :], [128, H, 130])
            if stop == "mixer":
                break
            ffn(32, G2, lambda j: act(xn[:, j, :], xT[:, j, :], AF.Square))
            if i == 0:
                dump("x3", xT[:, :, :], [128, KC, T])
            if stop == "ffn2":
                break
            bf = 6
            for j in range(NSUB):
                for k in range(KC):
                    mm(PS(bf, j, j + 1), xn[:, k, j * 128:(j + 1) * 128], onesb[:, 0:1], k == 0, k == KC - 1)
            act(frs[:, 0, :], PS(bf, 0, 4), AF.Ln, bias=EPS, scale=1.0 / D)
            act(frs[:, 1, :], frs[:, 0, :], AF.Exp, scale=-0.5)
            for j in range(NSUB):
                b2 = (0, 2, 4, 0)[j]
                for k in range(KC):
                    tr(ps[:, b2 * 512 + k * 128: b2 * 512 + (k + 1) * 128], xT[:, k, j * 128:(j + 1) * 128], ident32)
                so = stage_out[:, j, :]
                stt(so, ps[:, b2 * 512: b2 * 512 + 1024], frs[:, 1, j:j + 1], gfin[:, :], ALU.mult, ALU.mult)
                r0 = i * T + j * 128
                dma("pool", ch_out[j], out_d[r0:r0 + 128, :], so, reads=[so])

        P.finalize(nc, es)
    return nc, dbg_outs


def _kmaj(W, c0, n):
    K = W.shape[0]
    return W[:, c0:c0 + n].reshape(K // 128, 128, n).transpose(1, 0, 2)


def _pad(a):
    a = a.reshape(128, -1)
    out = np.zeros((128, 4096), np.float32)
    out[:, :a.shape[1]] = a
    return out


def _consts():
    c = np.zeros((128, NCST), np.float32)
    s = np.arange(128)[:, None]
    t = np.arange(128)[None, :]
    c[:, C_ID:C_ID + 128] = (s == t)
    c[:, C_TRI:C_TRI + 128] = (s <= t)
    c[:, C_ONE:C_ONE + 128] = 1.0
    for g, w in enumerate((2, 4, 8, 16)):
        inwin = ((t - s) >= 0) & ((t - s) <= w - 1)
        cur = inwin / w - (s == t)
        prev = ((t - s + 128) <= w - 1) / w
        cnt = np.minimum(t + 1, w)
        first = inwin / cnt - (s == t)
        c[:, C_BAND + (0 * 4 + g) * 128: C_BAND + (0 * 4 + g + 1) * 128] = cur
        c[:, C_BAND + (1 * 4 + g) * 128: C_BAND + (1 * 4 + g + 1) * 128] = prev
        c[:, C_BAND + (2 * 4 + g) * 128: C_BAND + (2 * 4 + g + 1) * 128] = first
    return c


def _layout(inp):
    f = lambda a: np.asarray(a, dtype=np.float32)
    pieces = np.zeros((NPIECE, 128, 4096), np.float32)

    def ffn_pieces(base, wgu, wd):
        for p in range(11):
            a = np.concatenate([_kmaj(wgu, 256 * p, 256), _kmaj(wgu, DFF + 256 * p, 256)], axis=2)
            pieces[base + p] = a.reshape(128, 4096)
        for j in range(8):
            pieces[base + 11 + j] = _pad(_kmaj(wd, 128 * j, 128))

    ffn_pieces(0, f(inp["ffn1_w_gu"])[0], f(inp["ffn1_w_down"])[0])
    ffn_pieces(32, f(inp["ffn2_w_gu"])[0], f(inp["ffn2_w_down"])[0])
    win = f(inp["w_in"])[0]
    for n, c0 in ((19, 0), (20, 512), (21, 1024), (22, 1536), (23, 2048), (24, 2560), (25, 3072), (26, 3584), (27, 4096)):
        pieces[n] = _kmaj(win, c0, 512).reshape(128, 4096)
    pieces[28] = _kmaj(f(inp["p_a"])[0], 0, 1024).reshape(128, 4096)
    pieces[29] = _kmaj(f(inp["p_b"])[0], 0, 1024).reshape(128, 4096)
    wo = f(inp["w_out"])[0]
    pieces[30] = _kmaj(wo, 0, 512).reshape(128, 4096)
    pieces[31] = _kmaj(wo, 512, 512).reshape(128, 4096)

    col = lambda v: f(v).reshape(-1, 128).T
    pvv = np.zeros((128, NPV), np.float32)
    b_in = f(inp["b_in"])[0]
    pvv[:, G1:G1 + 8] = col(inp["ffn1_norm_g"][0])
    pvv[:, GM:GM + 8] = col(inp["mix_norm_g"][0])
    pvv[:, G2:G2 + 8] = col(inp["ffn2_norm_g"][0])
    pvv[:, BQK:BQK + 8] = col(b_in[0:1024])
    pvv[:, BO:BO + 4] = col(b_in[1536:2048])
    pvv[:, BGA:BGA + 8] = col(b_in[2560:3584])
    pvv[:, BGB:BGB + 8] = col(b_in[3584:4608])
    pvv[:, HNG:HNG + 4] = col(inp["head_norm_g"][0])
    pvv[:, PSC:PSC + 4] = col(inp["pool_scale"][0])
    cw = f(inp["conv_qk"])[0]
    for j in range(4):
        pvv[:, CONV + j * 8: CONV + j * 8 + 8] = col(cw[j])
    rowp = np.concatenate([b_in[4608:4616], b_in[1024:1536], f(inp["final_norm_g"])]).reshape(1, -1)
    wsmv = np.zeros((128, 576), np.float32)
    wsmv[:, 0:64] = _kmaj(win, 4608, 8).reshape(128, 64)
    wsmv[:, 64:576] = f(inp["w_pool"])[0].transpose(1, 0, 2).reshape(128, 512)
    return {"wsrc": pieces, "pv": pvv, "rowp": np.ascontiguousarray(rowp), "wsm": wsmv, "cst": _consts()}


_NC_CACHE = {}


def kernel(**inputs):
    x = np.asarray(inputs["x"], dtype=np.float32)
    shared = _layout(inputs)
    nt = SEQ // T
    if nt not in _NC_CACHE:
        _NC_CACHE[nt] = build_nc(nt)[0]
    nc = _NC_CACHE[nt]
    in_maps = [dict(shared, x=np.ascontiguousarray(x[b])) for b in range(NB)]
    res = run_bass_kernel_spmd(nc, in_maps, core_ids=list(range(NB)))
    return np.stack([np.asarray(r["out"]) for r in res.results], axis=0).astype(np.float32)
```

```python
import numpy as np
from contextlib import ExitStack

import concourse.bass as bass
import concourse.mybir as mybir
from concourse.bass_utils import run_bass_kernel_spmd

F32 = mybir.dt.float32
BF16 = mybir.dt.bfloat16
AF = mybir.ActivationFunctionType
ALU = mybir.AluOpType

D = 1024
SEQ = 8192
NB = 8
T = 512
NSUB = 4
KC = 8
DFF = 2816
NFC = 22
H = 4
EPS = 1e-6
SCALE = 128.0 ** -0.5
NPIECE = 51
NSLOT = 5
NCONV = 8

G1, GM, G2, BQK, BO, BGA, BGB, HNG, PSC, CONV = 0, 8, 16, 24, 32, 36, 44, 52, 56, 60
NPV = 92
C_ID, C_TRI, C_ONE, C_BAND = 0, 128, 256, 384
NCST = 384 + 12 * 128


def _esz(dt):
    return 2 if dt == BF16 else 4


class Chan:
    def __init__(self, name, is_dma, eng=None):
        self.name = name
        self.is_dma = is_dma
        self.eng = eng
        self.ops = []
        self.sem = None


class Op:
    __slots__ = ("eng", "chan", "fn", "deps", "needs_inc", "idx", "value")


class Rec:
    __slots__ = ("lo", "hi", "writers", "readers", "ov")


class Prog:
    ENGS = ("pe", "act", "dve", "pool", "sp")

    def __init__(self):
        self.eng_ops = {e: [] for e in self.ENGS}
        self.dma_chans = []
        self.all_eng_chans = []
        self.epoch = -1
        self.new_epoch()
        self.state = {}
        self._bank = 0

    def new_epoch(self):
        self.epoch += 1
        self.eng_chan = {e: Chan(f"{e}{self.epoch}", False, e) for e in ("pe", "act", "dve", "pool")}
        self.all_eng_chans += list(self.eng_chan.values())

    def dma_chan(self, name):
        c = Chan(name, True)
        self.dma_chans.append(c)
        return c

    def _key(self, a):
        if isinstance(a, tuple):
            return a
        dims = a.ap
        pstep = dims[0][0]
        start = a.offset % pstep if pstep > 0 else a.offset
        lo = hi = start
        for step, cnt in dims[1:]:
            if step >= 0:
                hi += step * (cnt - 1)
            else:
                lo += step * (cnt - 1)
        e = _esz(a.dtype)
        lo, hi = lo * e, (hi + 1) * e
        if a.tensor.name == "ps":
            lo = (lo // 2048) * 2048
            hi = ((hi + 2047) // 2048) * 2048
        return (a.tensor.name, lo, hi)

    def _rec(self, key):
        name, lo, hi = key
        recs = self.state.setdefault(name, {})
        r = recs.get((lo, hi))
        if r is None:
            r = Rec()
            r.lo, r.hi, r.writers, r.readers, r.ov = lo, hi, {}, {}, []
            for o in recs.values():
                if o.lo < hi and lo < o.hi:
                    o.ov.append(r)
                    r.ov.append(o)
            r.ov.append(r)
            recs[(lo, hi)] = r
        return r

    def op(self, eng, fn, reads=(), writes=(), chan=None):
        o = Op()
        o.eng, o.fn, o.needs_inc, o.value = eng, fn, False, None
        o.chan = chan if chan is not None else self.eng_chan[eng]
        o.idx = len(o.chan.ops)
        deps = {}

        def add(d):
            cur = deps.get(d.chan)
            if cur is None or d.idx > cur.idx:
                deps[d.chan] = d

        if o.chan.is_dma and o.idx > 0:
            add(o.chan.ops[o.idx - 1])
        rkeys = [self._key(a) for a in reads]
        wkeys = [self._key(a) for a in writes]
        psk = [k for k in rkeys + wkeys if k[0] == "ps"]
        rkeys = [k for k in rkeys if k[0] != "ps"]
        wkeys = [k for k in wkeys if k[0] != "ps"]
        for k in psk:
            for b in range(k[1] // 2048, k[2] // 2048):
                kb = ("ps", b * 2048, (b + 1) * 2048)
                if kb not in wkeys:
                    wkeys.append(kb)
        rrecs = [self._rec(k) for k in rkeys]
        wrecs = [self._rec(k) for k in wkeys]
        for r in rrecs:
            for x in r.ov:
                for w in x.writers.values():
                    add(w)
        for r in wrecs:
            for x in r.ov:
                for w in x.writers.values():
                    add(w)
                for w in x.readers.values():
                    add(w)
        for r in rrecs:
            r.readers[o.chan] = o
        for r in wrecs:
            for x in r.ov:
                if x is r:
                    x.writers = {o.chan: o}
                    x.readers = {}
                else:
                    x.writers[o.chan] = o
        if eng == "pe":
            for ch in [c for c in deps if c.eng == "pe"]:
                del deps[ch]
        o.deps = deps
        o.chan.ops.append(o)
        self.eng_ops[eng].append(o)
        return o

    def bank(self):
        b = self._bank
        self._bank = (b + 1) % 7
        return b

    def bank2(self):
        b = self._bank
        while b % 2 or b >= 6:
            b = (b + 1) % 7
        self._bank = (b + 2) % 7
        return b

    def finalize(self, nc, es):
        for ops in self.eng_ops.values():
            for o in ops:
                for d in o.deps.values():
                    d.needs_inc = True
        for c in [c for c in self.all_eng_chans if c.ops] + self.dma_chans:
            c.sem = es.enter_context(nc.semaphore(c.name))
            n = 0
            for o in c.ops:
                if c.is_dma:
                    n += 16
                    o.needs_inc = True
                elif o.needs_inc:
                    n += 1
                o.value = n
        block = es.enter_context(nc.Block())
        names = {"pe": "tensor", "act": "scalar", "dve": "vector", "pool": "gpsimd", "sp": "sync"}

        def make(eng):
            ops = self.eng_ops[eng]

            def body(e):
                waited = {}
                last_dma = {}
                for o in ops:
                    for ch, d in o.deps.items():
                        if waited.get(ch, 0) < d.value:
                            e.wait_ge(ch.sem, d.value)
                            waited[ch] = d.value
                    ins = o.fn(e)
                    if o.needs_inc:
                        ins.then_inc(o.chan.sem, 16 if o.chan.is_dma else 1)
                    if o.chan.is_dma:
                        last_dma[o.chan] = o.value
                for ch, v in last_dma.items():
                    if waited.get(ch, 0) < v:
                        e.wait_ge(ch.sem, v)
            return body

        for eng in self.ENGS:
            if self.eng_ops[eng]:
                getattr(block, names[eng])(make(eng))


def build_nc(nt, first_tile_global=True, dbg=None, stop=None):
    nc = bass.Bass("TRN2", target_bir_lowering=False)
    ntok = nt * T
    x_d = nc.dram_tensor("x", [ntok, D], F32, kind="ExternalInput").ap()
    wsrc_d = nc.dram_tensor("wsrc", [NPIECE, 128, 4096], F32, kind="ExternalInput").ap()
    pv_d = nc.dram_tensor("pv", [128, NPV], F32, kind="ExternalInput").ap()
    rowp_d = nc.dram_tensor("rowp", [1, 8 + 512 + 1024], F32, kind="ExternalInput").ap()
    wsm_d = nc.dram_tensor("wsm", [128, 576], F32, kind="ExternalInput").ap()
    cst_d = nc.dram_tensor("cst", [128, NCST], F32, kind="ExternalInput").ap()
    out_d = nc.dram_tensor("out", [ntok, D], F32, kind="ExternalOutput").ap()
    scr_d = nc.dram_tensor("scr", [NPIECE, 128, 4096], BF16).ap()
    dbg_outs = {}

    P = Prog()
    es = ExitStack()
    with es:
        def sb(name, shape, dt):
            return es.enter_context(nc.sbuf_tensor(name, shape, dt))

        stage_in = sb("stage_in", [128, NSUB, D], F32)
        stage_out = sb("stage_out", [128, 4, D], F32)
        xT = sb("xT", [128, KC, T], F32)
        xn = sb("xn", [128, KC, T], BF16)
        rs = sb("rs", [128, 2, T], F32)
        hid = sb("hid", [128, NFC, T], BF16)
        tmpf = sb("tmpf", [128, 2, T], F32)
        tmpb = sb("tmpb", [128, 2, T], BF16)
        zqk = sb("zqk", [128, KC, 516], BF16)
        qkT = sb("qkT", [128, KC, T], BF16)
        ktm = sb("ktm", [128, NSUB, T], BF16)
        vaug = sb("vaug", [128, NSUB, H, 130], BF16)
        U = sb("U", [128, 5, T], BF16)
        poolT = sb("poolT", [128, 4, T], BF16)
        ybT = sb("ybT", [128, 4, T], BF16)
        spT = sb("spT", [128, 2, T], BF16)
        ytm = sb("ytm", [128, 2, T], BF16)
        yaT = sb("yaT", [128, H, T], BF16)
        Cf = sb("Cf", [128, H, 130], F32)
        Cbf = sb("Cbf", [128, H, 130], BF16)
        zif = sb("zif", [128, NSUB, 8], F32)
        ef = sb("ef", [128, NSUB, 4], F32)
        nlf = sb("nlf", [128, NSUB, 4], F32)
        eb = sb("eb", [128, NSUB, 4], F32)
        eB = sb("eB", [128, NSUB, 4], F32)
        inv2 = sb("inv2", [128, NSUB, 4], F32)
        tmpi = sb("tmpi", [128, NSUB, 4], F32)
        wgt = sb("wgt", [128, NSUB, 4], F32)
        sc = sb("sc", [128, 10, 4], F32)
        ss = sb("ss", [128, NSUB, 4], F32)
        frs = sb("frs", [128, 2, 4], F32)
        junk = sb("junk", [128, 128], BF16)
        dmy = sb("dmy", [128, 4], F32)
        cbf = sb("cbf", [128, NCST], BF16)
        c32 = sb("c32", [128, 384], F32)
        diag = sb("diag", [128, 32, 128], BF16)
        wsm = sb("wsm_s", [128, 576], BF16)
        pv = sb("pv_s", [128, NPV], F32)
        bif = sb("bif", [128, 8], F32)
        bv = sb("bv", [1, 512], BF16)
        gfin = sb("gfin", [128, D], F32)
        wslot = [sb(f"wslot{i}", [128, 4096], BF16) for i in range(NSLOT)]
        ps = es.enter_context(nc.psum_tensor("ps", [128, 4096], F32))
        psb = ps[:, :].bitcast(BF16)

        def PS(b, lo=0, hi=512):
            return ps[:, b * 512 + lo: b * 512 + hi]

        def PSB(b, lo, hi):
            return psb[:, b * 1024 + lo: b * 1024 + hi]

        ident32 = c32[:, C_ID:C_ID + 128]
        tri32 = c32[:, C_TRI:C_TRI + 128]
        ones32 = c32[:, C_ONE:C_ONE + 128]
        identb = cbf[:, C_ID:C_ID + 128]
        trib = cbf[:, C_TRI:C_TRI + 128]
        onesb = cbf[:, C_ONE:C_ONE + 128]

        def band(kind, g):
            o = C_BAND + (kind * 4 + g) * 128
            return cbf[:, o:o + 128]

        def pcol(c, n=1):
            return pv[:, c:c + n]

        def mm(out, lhsT, rhs, start, stop):
            P.op("pe", lambda e: e.matmul(out, lhsT, rhs, start=start, stop=stop),
                 reads=[lhsT, rhs], writes=[out])

        def tr(out, in_, ident):
            P.op("pe", lambda e: e.transpose(out, in_, ident), reads=[in_, ident], writes=[out])

        def act(out, in_, func, bias=None, scale=None, accum=None):
            kw = {}
            rd = [in_]
            wr = [out]
            if bias is not None:
                kw["bias"] = bias
                if not isinstance(bias, float):
                    rd.append(bias)
            if scale is not None:
                kw["scale"] = scale
                if not isinstance(scale, float):
                    rd.append(scale)
            if accum is not None:
                kw["accum_out"] = accum
                wr.append(accum)
            P.op("act", lambda e: e.activation(out, in_, func, **kw), reads=rd, writes=wr)

        def tt(out, in0, in1, op, eng="dve"):
            P.op(eng, lambda e: e.tensor_tensor(out, in0, in1, op), reads=[in0, in1], writes=[out])

        def stt(out, in0, scalar, in1, op0, op1):
            rd = [in0, in1] + ([] if isinstance(scalar, float) else [scalar])
            P.op("dve", lambda e: e.scalar_tensor_tensor(out, in0, scalar, in1, op0, op1),
                 reads=rd, writes=[out])

        def ts(out, in0, s1, op0, s2=None, op1=None):
            rd = [in0] + ([] if isinstance(s1, float) else [s1])
            if op1 is None:
                P.op("dve", lambda e: e.tensor_single_scalar(out, in0, s1, op0), reads=rd, writes=[out])
            else:
                P.op("dve", lambda e: e.tensor_scalar(out, in0, s1, s2, op0, op1), reads=rd, writes=[out])

        def cp(out, in_, eng="dve"):
            if eng == "act":
                P.op("act", lambda e: e.copy(out, in_), reads=[in_], writes=[out])
            else:
                P.op(eng, lambda e: e.tensor_copy(out, in_), reads=[in_], writes=[out])

        def memset(ap, v, eng="dve"):
            P.op(eng, lambda e: e.memset(ap, v), writes=[ap])

        def dma(eng, chan, out, in_, reads=(), writes=()):
            P.op(eng, lambda e: e.dma_start(out=out, in_=in_), reads=list(reads), writes=list(writes), chan=chan)

        def dump(name, ap, shape, dt=F32):
            if dbg is None or name not in dbg:
                return
            d = nc.dram_tensor("dbg_" + name, list(shape), dt, kind="ExternalOutput").ap()
            dbg_outs[name] = d
            dma("pool", P.dma_chan("dbg_" + name), d, ap, reads=[ap])

        ch_xin = P.dma_chan("xin")
        ch_out = [P.dma_chan(f"out{q}") for q in range(4)]
        ch_w = [P.dma_chan(f"w{i}") for i in range(NSLOT)]
        ch_st = [P.dma_chan(f"st{i}") for i in range(NSLOT)]
        ch_wc = [P.dma_chan(f"wc{i}") for i in range(NSLOT)]

        def load_x(i):
            src = x_d[i * T:(i + 1) * T, :].rearrange("(j p) d -> p j d", p=128)
            dma("pool", ch_xin, stage_in[:, :, :], src, writes=[stage_in[:, :, :]])

        for nm, dst, src in (
            ("pv", pv[:, :], pv_d),
            ("c32", c32[:, :], cst_d[:, 0:384]),
            ("cbf", cbf[:, :], cst_d),
            ("wsm", wsm[:, :], wsm_d),
            ("bif", bif[:, :], rowp_d[:, 0:8].partition_broadcast(128)),
            ("bv", bv[:, :], rowp_d[:, 8:520]),
            ("gfin", gfin[:, :], rowp_d[:, 520:1544].partition_broadcast(128)),
        ):
            dma("pool", P.dma_chan("su_" + nm), dst, src, writes=[dst])
        load_x(0)
        memset(dmy[:, :], 1.0)
        memset(Cf[:, :, :], 0.0)
        memset(Cbf[:, :, :], 0.0)
        memset(zqk[:, :, 0:3], 0.0)
        memset(U[:, 0, :], 0.0)
        memset(vaug[:, :, :, :], 0.0)
        for j in range(4):
            for ch in range(KC):
                ts(diag[:, j * 8 + ch, :], identb, pcol(CONV + j * 8 + ch), ALU.mult)

        wstate = {"n": 0}

        converted = set()

        def load_piece(k):
            s = wstate["n"] % NSLOT
            wstate["n"] += 1
            nco = 2816 if (11 <= k < 19 or 43 <= k < 51) else 4096
            if k not in converted:
                converted.add(k)
                dma("pool", ch_wc[s], wslot[s][:, 0:nco], wsrc_d[k][:, 0:nco], writes=[wslot[s][:, :]])
                dma("sp", ch_st[s], scr_d[k][:, 0:nco], wslot[s][:, 0:nco], reads=[wslot[s][:, :]], writes=[("scr", k, k + 1)])
            else:
                dma("sp", ch_w[s], wslot[s][:, 0:nco], scr_d[k][:, 0:nco], reads=[("scr", k, k + 1)], writes=[wslot[s][:, :]])
            return wslot[s]

        def preload_ln():
            act(dmy[:, 2:3], dmy[:, 0:1], AF.Ln)

        NBANK = 7

        sq_pending = []

        def sq_flush():
            while sq_pending:
                k, buf = sq_pending.pop(0)
                mm(PS(NBANK), onesb, buf[:, k, :], k == 0, k == KC - 1)

        def sq_chunk(k, buf):
            act(buf[:, k, :], xT[:, k, :], AF.Square)
            sq_flush()
            sq_pending.append((k, buf))

        def norm_finish(gcol):
            sq_flush()
            act(rs[:, 0, :], PS(NBANK), AF.Ln, bias=EPS, scale=1.0 / D)
            act(rs[:, 1, :], rs[:, 0, :], AF.Exp, scale=-0.5)
            for k in range(KC):
                stt(xn[:, k, :], xT[:, k, :], pcol(gcol + k), rs[:, 1, :], ALU.mult, ALU.mult)

        def ffn(piece0, gcol, after_chunk):
            norm_finish(gcol)
            for p in range(11):
                w = load_piece(piece0 + p)
                for jj in range(2):
                    j = 2 * p + jj
                    bg = P.bank()
                    for k in range(KC):
                        mm(PS(bg), w[:, k * 512 + jj * 128: k * 512 + jj * 128 + 128], xn[:, k, :], k == 0, k == KC - 1)
                    bu = P.bank()
                    for k in range(KC):
                        mm(PS(bu), w[:, k * 512 + 256 + jj * 128: k * 512 + 256 + jj * 128 + 128], xn[:, k, :], k == 0, k == KC - 1)
                    act(tmpf[:, j % 2, :], PS(bg), AF.Silu)
                    tt(hid[:, j, :], tmpf[:, j % 2, :], PS(bu), ALU.mult)
            preload_ln()
            for j in range(KC):
                w = load_piece(piece0 + 11 + j)
                b = P.bank()
                for k in range(NFC):
                    mm(PS(b), w[:, k * 128:(k + 1) * 128], hid[:, k, :], k == 0, k == NFC - 1)
                stt(xT[:, j, :], PS(b), 0.5, xT[:, j, :], ALU.mult, ALU.add)
                after_chunk(j)

        def proj_fm(w, cc, evac, bank=None):
            b = P.bank() if bank is None else bank
            for k in range(KC):
                mm(PS(b), w[:, k * 512 + cc * 128: k * 512 + cc * 128 + 128], xn[:, k, :], k == 0, k == KC - 1)
            evac(b)

        def mixer(i):
            norm_finish(GM)
            bgt = P.bank()
            for j in range(NSUB):
                for k in range(KC):
                    mm(PS(bgt, j * 8, j * 8 + 8), xn[:, k, j * 128:(j + 1) * 128], wsm[:, k * 8:(k + 1) * 8], k == 0, k == KC - 1)
            tt(zif[:, :, :], PS(bgt, 0, 32).rearrange("p (j c) -> p j c", j=NSUB),
               bif[:, :].unsqueeze(1).to_broadcast([128, NSUB, 8]), ALU.add)
            act(ef[:, :, :], zif[:, :, 4:8], AF.Exp, scale=-1.0)
            act(nlf[:, :, :], ef[:, :, :], AF.Ln, bias=1.0)
            bcs = P.bank()
            for j in range(NSUB):
                mm(PS(bcs, j * 4, j * 4 + 4), tri32, nlf[:, j, :], True, True)
            for j in range(NSUB):
                mm(PS(bcs, 16 + j * 4, 16 + j * 4 + 4), ones32, nlf[:, j, :], True, True)
            act(eb[:, :, :], PS(bcs, 0, 16).rearrange("p (j c) -> p j c", j=NSUB), AF.Exp, scale=-1.0)
            act(eB[:, :, :], PS(bcs, 16, 32).rearrange("p (j c) -> p j c", j=NSUB), AF.Exp, scale=-1.0)
            act(inv2[:, :, :], PS(bcs, 0, 16).rearrange("p (j c) -> p j c", j=NSUB), AF.Exp, scale=2.0)
            tt(tmpi[:, :, :], zif[:, :, 0:4], PS(bcs, 0, 16).rearrange("p (j c) -> p j c", j=NSUB), ALU.add)
            act(wgt[:, :, :], tmpi[:, :, :], AF.Exp)
            cp(vaug[:, :, :, 128:129], wgt[:, :, :].unsqueeze(3))
            if stop == "mx_gates":
                return True
            for pc in range(2):
                w = load_piece(19 + pc)
                for cc in range(4):
                    ch = pc * 4 + cc
                    proj_fm(w, cc, lambda b, ch=ch: ts(zqk[:, ch, 3:515], PS(b), pcol(BQK + ch), ALU.add))
            if stop == "mx_qk":
                return True
            for ch in range(KC):
                b = P.bank()
                for j in range(4):
                    mm(PS(b), diag[:, j * 8 + ch, :], zqk[:, ch, j:j + 512], j == 0, j == 3)
                act(qkT[:, ch, :], PS(b), AF.Silu)
            cp(zqk[:, :, 0:3], zqk[:, :, 512:515])
            if stop == "mx_conv":
                return True
            for j in range(NSUB):
                b = P.bank()
                for h in range(H):
                    tr(PSB(b, h * 128, (h + 1) * 128), qkT[:, 4 + h, j * 128:(j + 1) * 128], identb)
                cp(ktm[:, j, :], PSB(b, 0, 512), eng="act")
            if stop == "mx_ktr":
                return True
            w = load_piece(21)
            for j in range(NSUB):
                b = P.bank()
                for k in range(KC):
                    mm(PS(b), xn[:, k, j * 128:(j + 1) * 128], w[:, k * 512:(k + 1) * 512], k == 0, False)
                mm(PS(b), cbf[0:1, C_ONE:C_ONE + 128], bv[0:1, :], False, True)
                tt(vaug[:, j, :, 0:128], PS(b).rearrange("p (h v) -> p h v", h=H),
                   wgt[:, j, :].unsqueeze(2).to_broadcast([128, H, 128]), ALU.mult)
            if stop == "mx_v":
                return True
            w = load_piece(22)
            for cc in range(4):
                proj_fm(w, cc, lambda b, cc=cc: act(hid[:, 16 + cc, :], PS(b), AF.Sigmoid, bias=pcol(BO + cc)))
            if stop == "mx_o":
                return True
            preload_ln()
            if i > 0:
                cp(U[:, 0, :], U[:, 4, :])
            w = load_piece(23)
            for j in range(NSUB):
                b = P.bank()
                for k in range(KC):
                    mm(PS(b), xn[:, k, j * 128:(j + 1) * 128], w[:, k * 512:(k + 1) * 512], k == 0, k == KC - 1)
                cp(U[:, 1 + j, :], PS(b), eng="act")
            for g in range(4):
                b = P.bank()
                for j in range(NSUB):
                    first = first_tile_global and i == 0 and j == 0
                    mm(PS(b, j * 128, (j + 1) * 128), U[:, j, g * 128:(g + 1) * 128], band(1, g), True, False)
                    mm(PS(b, j * 128, (j + 1) * 128), U[:, j + 1, g * 128:(g + 1) * 128], band(2 if first else 0, g), False, True)
                cp(poolT[:, g, :], PS(b))
            for g in range(4):
                b = P.bank()
                mm(PS(b), wsm[:, 64 + g * 128: 64 + (g + 1) * 128], poolT[:, g, :], True, True)
                act(ybT[:, g, :], PS(b), AF.Copy, scale=pcol(PSC + g))
            if stop == "mx_pool":
                return True
            gate_w = {}

            def gate_filler(which, c):
                def run(bank=None):
                    pidx = (24 if which == 0 else 26) + c // 4
                    if pidx not in gate_w:
                        gate_w[pidx] = load_piece(pidx)
                    bcol = (BGA if which == 0 else BGB) + c
                    dst = hid[:, (0 if which == 0 else 8) + c, :]
                    proj_fm(gate_w[pidx], c % 4, lambda b: act(dst, PS(b), AF.Identity, bias=pcol(bcol)), bank=bank)
                return run

            fillers = [gate_filler(wh, c) for wh in range(2) for c in range(KC)]

            def fill():
                if fillers:
                    fillers.pop(0)()

            memset(ss[:, :, :], 0.0)
            small = {"n": 0}

            def small_bank():
                small["n"] += 1
                return 6 + small["n"] % 2

            ACCB = (0, 2)
            CBANK = 4
            S_ = lambda n: sc[:, n, :]

            def st_S(j):
                tok = slice(j * 128, (j + 1) * 128)
                bS = small_bank()
                for h in range(H):
                    mm(PS(bS, h * 128, (h + 1) * 128), qkT[:, 4 + h, tok], qkT[:, h, tok], True, True)
                tt(spT[:, j % 2, :].rearrange("p (h t) -> p h t", h=H), PS(bS).rearrange("p (h t) -> p h t", h=H),
                   trib.unsqueeze(1).to_broadcast([128, H, 128]), ALU.mult)

            def st_A(j):
                tok = slice(j * 128, (j + 1) * 128)
                par = j % 2
                bA = ACCB[par]
                for h in range(H):
                    o_ = ps[:, bA * 512 + h * 256: bA * 512 + h * 256 + 130]
                    mm(o_, spT[:, par, h * 128:(h + 1) * 128], vaug[:, j, h, 0:130], True, False)
                    mm(o_, qkT[:, h, tok], Cbf[:, h, 0:130], False, True)
                acc = ps[:, bA * 512: bA * 512 + 1024].rearrange("p (h c) -> p h c", h=H)
                for h in range(H):
                    act(junk[:, :], acc[:, h, 0:128], AF.Square, accum=ss[:, j, h:h + 1])
                bC = CBANK
                for h in range(H):
                    mm(ps[:, bC * 512 + h * 256: bC * 512 + h * 256 + 130], ktm[:, j, h * 128:(h + 1) * 128], vaug[:, j, h, 0:130], True, True)
                accC = ps[:, bC * 512: bC * 512 + 1024].rearrange("p (h c) -> p h c", h=H)
                tt(Cf[:, :, :], Cf[:, :, :], accC[:, :, 0:130], ALU.add)
                tt(Cf[:, :, :], Cf[:, :, :], eB[:, j, :].unsqueeze(2).to_broadcast([128, H, 130]), ALU.mult)
                cp(Cbf[:, :, :], Cf[:, :, :])

            def st_scal(j):
                par = j % 2
                bA = ACCB[par]
                acc = ps[:, bA * 512: bA * 512 + 1024].rearrange("p (h c) -> p h c", h=H)
                stt(S_(0).unsqueeze(2), acc[:, :, 128:129], SCALE, eb[:, j, :].unsqueeze(2), ALU.mult, ALU.mult)
                tt(S_(1), S_(0), S_(0), ALU.mult)
                ts(S_(2), S_(1), 1.0, ALU.max, EPS / (SCALE * SCALE), ALU.mult)
                tt(S_(3), S_(2), inv2[:, j, :], ALU.mult)
                stt(S_(4), ss[:, j, :], 1.0 / 128.0, S_(3), ALU.mult, ALU.add)
                act(S_(5), S_(4), AF.Ln)
                act(S_(6), S_(5), AF.Exp, scale=-0.5)
                tt(ytm[:, par, :].rearrange("p (h v) -> p h v", h=H), acc[:, :, 0:128],
                   S_(6).unsqueeze(2).to_broadcast([128, H, 128]), ALU.mult)

            def st_T(j):
                tok = slice(j * 128, (j + 1) * 128)
                par = j % 2
                bT = small_bank()
                for h in range(H):
                    tr(PSB(bT, h * 128, (h + 1) * 128), ytm[:, par, h * 128:(h + 1) * 128], identb)
                for h in range(H):
                    stt(yaT[:, h, tok], PSB(bT, h * 128, (h + 1) * 128), pcol(HNG + h), hid[:, 16 + h, tok], ALU.mult, ALU.mult)

            def fillS():
                if fillers:
                    fillers.pop(0)(small_bank())

            st_S(0)
            fillS()
            st_A(0)
            fillS()
            for j in range(NSUB):
                if j + 1 < NSUB:
                    st_S(j + 1)
                    fillS()
                st_scal(j)
                if j + 1 < NSUB:
                    st_A(j + 1)
                    fillS()
                if j >= 1:
                    st_T(j - 1)
                    fillS()
            st_T(NSUB - 1)
            if stop == "mx_lstm":
                return True
            while fillers:
                fillers.pop(0)()
            for c in range(KC):
                act(hid[:, c, :], hid[:, c, :], AF.Sigmoid)
                act(hid[:, 8 + c, :], hid[:, 8 + c, :], AF.Sigmoid)
            preload_ln()
            wa = load_piece(28)
            wb = load_piece(29)
            for c in range(KC):
                ba = P.bank()
                for k in range(4):
                    mm(PS(ba), wa[:, k * 1024 + c * 128: k * 1024 + c * 128 + 128], yaT[:, k, :], k == 0, k == 3)
                bb = P.bank()
                for k in range(4):
                    mm(PS(bb), wb[:, k * 1024 + c * 128: k * 1024 + c * 128 + 128], ybT[:, k, :], k == 0, k == 3)
                tt(tmpb[:, 0, :], PS(ba), hid[:, c, :], ALU.mult)
                tt(tmpb[:, 1, :], PS(bb), hid[:, 8 + c, :], ALU.mult)
                tt(xn[:, c, :], tmpb[:, 0, :], tmpb[:, 1, :], ALU.add)
            if stop == "mx_merge":
                return True
            for pc in range(2):
                w = load_piece(30 + pc)
                for cc in range(4):
                    c = pc * 4 + cc
                    proj_fm(w, cc, lambda b, c=c: tt(xT[:, c, :], PS(b), xT[:, c, :], ALU.add))
                    sq_chunk(c, qkT)

        for i in range(nt if stop != "setup" else 0):
            if i > 0:
                P.new_epoch()
            for k in range(KC):
                b = (6, 2, 3, 4, 5, 6, 2, 3)[k]
                for j in range(NSUB):
                    tr(PS(b, j * 128, (j + 1) * 128), stage_in[:, j, k * 128:(k + 1) * 128], ident32)
                cp(xT[:, k, :], PS(b), eng=("act" if (k < 4 or k % 2) else "dve"))
                sq_chunk(k, xn)
            if i + 1 < nt:
                load_x(i + 1)
            if i == 0:
                dump("x0", xT[:, :, :], [128, KC, T])
            if stop == "input":
                break
            ffn(0, G1, lambda j: sq_chunk(j, xn))
            if i == 0:
                dump("x1", xT[:, :, :], [128, KC, T])
            if stop == "ffn1":
                break
            if mixer(i):
                break
            if i == 0:
                dump("x2", xT[:, :, :], [128, KC, T])
                dump("qkT", qkT[:, :, :], [128, KC, T], BF16)
                dump("yaT", yaT[:, :, :], [128, H, T], BF16)
                dump("ybT", ybT[:, :, :], [128, 4, T], BF16)
                dump("vaug", vaug[:, :, :, :], [128, NSUB, H, 130], BF16)
                dump("eb", eb[:, :, :], [128, NSUB, 4])
                dump("wgt", wgt[:, :, :], [128, NSUB, 4])
                dump("Cf", Cf[:, :, :], [128, H, 130])
            if stop == "mixer":
                break
            ffn(32, G2, lambda j: act(xn[:, j, :], xT[:, j, :], AF.Square))
            if i == 0:
                dump("x3", xT[:, :, :], [128, KC, T])
            if stop == "ffn2":
                break
            bf = 6
            for j in range(NSUB):
                for k in range(KC):
                    mm(PS(bf, j, j + 1), xn[:, k, j * 128:(j + 1) * 128], onesb[:, 0:1], k == 0, k == KC - 1)
            act(frs[:, 0, :], PS(bf, 0, 4), AF.Ln, bias=EPS, scale=1.0 / D)
            act(frs[:, 1, :], frs[:, 0, :], AF.Exp, scale=-0.5)
            for j in range(NSUB):
                b2 = (0, 2, 4, 0)[j]
                for k in range(KC):
                    tr(ps[:, b2 * 512 + k * 128: b2 * 512 + (k + 1) * 128], xT[:, k, j * 128:(j + 1) * 128], ident32)
                so = stage_out[:, j, :]
                stt(so, ps[:, b2 * 512: b2 * 512 + 1024], frs[:, 1, j:j + 1], gfin[:, :], ALU.mult, ALU.mult)
                r0 = i * T + j * 128
                dma("pool", ch_out[j], out_d[r0:r0 + 128, :], so, reads=[so])

        P.finalize(nc, es)
    return nc, dbg_outs


def _kmaj(W, c0, n):
    K = W.shape[0]
    return W[:, c0:c0 + n].reshape(K // 128, 128, n).transpose(1, 0, 2)


def _pad(a):
    a = a.reshape(128, -1)
    out = np.zeros((128, 4096), np.float32)
    out[:, :a.shape[1]] = a
    return out


def _consts():
    c = np.zeros((128, NCST), np.float32)
    s = np.arange(128)[:, None]
    t = np.arange(128)[None, :]
    c[:, C_ID:C_ID + 128] = (s == t)
    c[:, C_TRI:C_TRI + 128] = (s <= t)
    c[:, C_ONE:C_ONE + 128] = 1.0
    for g, w in enumerate((2, 4, 8, 16)):
        inwin = ((t - s) >= 0) & ((t - s) <= w - 1)
        cur = inwin / w - (s == t)
        prev = ((t - s + 128) <= w - 1) / w
        cnt = np.minimum(t + 1, w)
        first = inwin / cnt - (s == t)
        c[:, C_BAND + (0 * 4 + g) * 128: C_BAND + (0 * 4 + g + 1) * 128] = cur
        c[:, C_BAND + (1 * 4 + g) * 128: C_BAND + (1 * 4 + g + 1) * 128] = prev
        c[:, C_BAND + (2 * 4 + g) * 128: C_BAND + (2 * 4 + g + 1) * 128] = first
    return c


def _layout(inp):
    f = lambda a: np.asarray(a, dtype=np.float32)
    pieces = np.zeros((NPIECE, 128, 4096), np.float32)

    def ffn_pieces(base, wgu, wd):
        for p in range(11):
            a = np.concatenate([_kmaj(wgu, 256 * p, 256), _kmaj(wgu, DFF + 256 * p, 256)], axis=2)
            pieces[base + p] = a.reshape(128, 4096)
        for j in range(8):
            pieces[base + 11 + j] = _pad(_kmaj(wd, 128 * j, 128))

    ffn_pieces(0, f(inp["ffn1_w_gu"])[0], f(inp["ffn1_w_down"])[0])
    ffn_pieces(32, f(inp["ffn2_w_gu"])[0], f(inp["ffn2_w_down"])[0])
    win = f(inp["w_in"])[0]
    for n, c0 in ((19, 0), (20, 512), (21, 1024), (22, 1536), (23, 2048), (24, 2560), (25, 3072), (26, 3584), (27, 4096)):
        pieces[n] = _kmaj(win, c0, 512).reshape(128, 4096)
    pieces[28] = _kmaj(f(inp["p_a"])[0], 0, 1024).reshape(128, 4096)
    pieces[29] = _kmaj(f(inp["p_b"])[0], 0, 1024).reshape(128, 4096)
    wo = f(inp["w_out"])[0]
    pieces[30] = _kmaj(wo, 0, 512).reshape(128, 4096)
    pieces[31] = _kmaj(wo, 512, 512).reshape(128, 4096)

    col = lambda v: f(v).reshape(-1, 128).T
    pvv = np.zeros((128, NPV), np.float32)
    b_in = f(inp["b_in"])[0]
    pvv[:, G1:G1 + 8] = col(inp["ffn1_norm_g"][0])
    pvv[:, GM:GM + 8] = col(inp["mix_norm_g"][0])
    pvv[:, G2:G2 + 8] = col(inp["ffn2_norm_g"][0])
    pvv[:, BQK:BQK + 8] = col(b_in[0:1024])
    pvv[:, BO:BO + 4] = col(b_in[1536:2048])
    pvv[:, BGA:BGA + 8] = col(b_in[2560:3584])
    pvv[:, BGB:BGB + 8] = col(b_in[3584:4608])
    pvv[:, HNG:HNG + 4] = col(inp["head_norm_g"][0])
    pvv[:, PSC:PSC + 4] = col(inp["pool_scale"][0])
    cw = f(inp["conv_qk"])[0]
    for j in range(4):
        pvv[:, CONV + j * 8: CONV + j * 8 + 8] = col(cw[j])
    rowp = np.concatenate([b_in[4608:4616], b_in[1024:1536], f(inp["final_norm_g"])]).reshape(1, -1)
    wsmv = np.zeros((128, 576), np.float32)
    wsmv[:, 0:64] = _kmaj(win, 4608, 8).reshape(128, 64)
    wsmv[:, 64:576] = f(inp["w_pool"])[0].transpose(1, 0, 2).reshape(128, 512)
    return {"wsrc": pieces, "pv": pvv, "rowp": np.ascontiguousarray(rowp), "wsm": wsmv, "cst": _consts()}


_NC_CACHE = {}


def kernel(**inputs):
    x = np.asarray(inputs["x"], dtype=np.float32)
    shared = _layout(inputs)
    nt = SEQ // T
    if nt not in _NC_CACHE:
        _NC_CACHE[nt] = build_nc(nt)[0]
    nc = _NC_CACHE[nt]
    in_maps = [dict(shared, x=np.ascontiguousarray(x[b])) for b in range(NB)]
    res = run_bass_kernel_spmd(nc, in_maps, core_ids=list(range(NB)))
    return np.stack([np.asarray(r["out"]) for r in res.results], axis=0).astype(np.float32)
```

```python
import numpy as np
from contextlib import ExitStack

import concourse.bass as bass
import concourse.mybir as mybir
from concourse.bass_utils import run_bass_kernel_spmd

F32 = mybir.dt.float32
BF16 = mybir.dt.bfloat16
AF = mybir.ActivationFunctionType
ALU = mybir.AluOpType

D = 1024
SEQ = 8192
NB = 8
T = 512
NSUB = 4
KC = 8
DFF = 2816
NFC = 22
H = 4
EPS = 1e-6
SCALE = 128.0 ** -0.5
NPIECE = 51
NSLOT = 5
NCONV = 8

G1, GM, G2, BQK, BO, BGA, BGB, HNG, PSC, CONV = 0, 8, 16, 24, 32, 36, 44, 52, 56, 60
NPV = 92
C_ID, C_TRI, C_ONE, C_BAND = 0, 128, 256, 384
NCST = 384 + 12 * 128


def _esz(dt):
    return 2 if dt == BF16 else 4


class Chan:
    def __init__(self, name, is_dma, eng=None):
        self.name = name
        self.is_dma = is_dma
        self.eng = eng
        self.ops = []
        self.sem = None


class Op:
    __slots__ = ("eng", "chan", "fn", "deps", "needs_inc", "idx", "value")


class Rec:
    __slots__ = ("lo", "hi", "writers", "readers", "ov")


class Prog:
    ENGS = ("pe", "act", "dve", "pool", "sp")

    def __init__(self):
        self.eng_ops = {e: [] for e in self.ENGS}
        self.dma_chans = []
        self.all_eng_chans = []
        self.epoch = -1
        self.new_epoch()
        self.state = {}
        self._bank = 0

    def new_epoch(self):
        self.epoch += 1
        self.eng_chan = {e: Chan(f"{e}{self.epoch}", False, e) for e in ("pe", "act", "dve", "pool")}
        self.all_eng_chans += list(self.eng_chan.values())

    def dma_chan(self, name):
        c = Chan(name, True)
        self.dma_chans.append(c)
        return c

    def _key(self, a):
        if isinstance(a, tuple):
            return a
        dims = a.ap
        pstep = dims[0][0]
        start = a.offset % pstep if pstep > 0 else a.offset
        lo = hi = start
        for step, cnt in dims[1:]:
            if step >= 0:
                hi += step * (cnt - 1)
            else:
                lo += step * (cnt - 1)
        e = _esz(a.dtype)
        lo, hi = lo * e, (hi + 1) * e
        if a.tensor.name == "ps":
            lo = (lo // 2048) * 2048
            hi = ((hi + 2047) // 2048) * 2048
        return (a.tensor.name, lo, hi)

    def _rec(self, key):
        name, lo, hi = key
        recs = self.state.setdefault(name, {})
        r = recs.get((lo, hi))
        if r is None:
            r = Rec()
            r.lo, r.hi, r.writers, r.readers, r.ov = lo, hi, {}, {}, []
            for o in recs.values():
                if o.lo < hi and lo < o.hi:
                    o.ov.append(r)
                    r.ov.append(o)
            r.ov.append(r)
            recs[(lo, hi)] = r
        return r

    def op(self, eng, fn, reads=(), writes=(), chan=None):
        o = Op()
        o.eng, o.fn, o.needs_inc, o.value = eng, fn, False, None
        o.chan = chan if chan is not None else self.eng_chan[eng]
        o.idx = len(o.chan.ops)
        deps = {}

        def add(d):
            cur = deps.get(d.chan)
            if cur is None or d.idx > cur.idx:
                deps[d.chan] = d

        if o.chan.is_dma and o.idx > 0:
            add(o.chan.ops[o.idx - 1])
        rkeys = [self._key(a) for a in reads]
        wkeys = [self._key(a) for a in writes]
        psk = [k for k in rkeys + wkeys if k[0] == "ps"]
        rkeys = [k for k in rkeys if k[0] != "ps"]
        wkeys = [k for k in wkeys if k[0] != "ps"]
        for k in psk:
            for b in range(k[1] // 2048, k[2] // 2048):
                kb = ("ps", b * 2048, (b + 1) * 2048)
                if kb not in wkeys:
                    wkeys.append(kb)
        rrecs = [self._rec(k) for k in rkeys]
        wrecs = [self._rec(k) for k in wkeys]
        for r in rrecs:
            for x in r.ov:
                for w in x.writers.values():
                    add(w)
        for r in wrecs:
            for x in r.ov:
                for w in x.writers.values():
                    add(w)
                for w in x.readers.values():
                    add(w)
        for r in rrecs:
            r.readers[o.chan] = o
        for r in wrecs:
            for x in r.ov:
                if x is r:
                    x.writers = {o.chan: o}
                    x.readers = {}
                else:
                    x.writers[o.chan] = o
        if eng == "pe":
            for ch in [c for c in deps if c.eng == "pe"]:
                del deps[ch]
        o.deps = deps
        o.chan.ops.append(o)
        self.eng_ops[eng].append(o)
        return o

    def bank(self):
        b = self._bank
        self._bank = (b + 1) % 7
        return b

    def bank2(self):
        b = self._bank
        while b % 2 or b >= 6:
            b = (b + 1) % 7
        self._bank = (b + 2) % 7
        return b

    def finalize(self, nc, es):
        for ops in self.eng_ops.values():
            for o in ops:
                for d in o.deps.values():
                    d.needs_inc = True
        for c in [c for c in self.all_eng_chans if c.ops] + self.dma_chans:
            c.sem = es.enter_context(nc.semaphore(c.name))
            n = 0
            for o in c.ops:
                if c.is_dma:
                    n += 16
                    o.needs_inc = True
                elif o.needs_inc:
                    n += 1
                o.value = n
        block = es.enter_context(nc.Block())
        names = {"pe": "tensor", "act": "scalar", "dve": "vector", "pool": "gpsimd", "sp": "sync"}

        def make(eng):
            ops = self.eng_ops[eng]

            def body(e):
                waited = {}
                last_dma = {}
                for o in ops:
                    for ch, d in o.deps.items():
                        if waited.get(ch, 0) < d.value:
                            e.wait_ge(ch.sem, d.value)
                            waited[ch] = d.value
                    ins = o.fn(e)
                    if o.needs_inc:
                        ins.then_inc(o.chan.sem, 16 if o.chan.is_dma else 1)
                    if o.chan.is_dma:
                        last_dma[o.chan] = o.value
                for ch, v in last_dma.items():
                    if waited.get(ch, 0) < v:
                        e.wait_ge(ch.sem, v)
            return body

        for eng in self.ENGS:
            if self.eng_ops[eng]:
                getattr(block, names[eng])(make(eng))


def build_nc(nt, first_tile_global=True, dbg=None, stop=None):
    nc = bass.Bass("TRN2", target_bir_lowering=False)
    ntok = nt * T
    x_d = nc.dram_tensor("x", [ntok, D], F32, kind="ExternalInput").ap()
    wsrc_d = nc.dram_tensor("wsrc", [NPIECE, 128, 4096], F32, kind="ExternalInput").ap()
    pv_d = nc.dram_tensor("pv", [128, NPV], F32, kind="ExternalInput").ap()
    rowp_d = nc.dram_tensor("rowp", [1, 8 + 512 + 1024], F32, kind="ExternalInput").ap()
    wsm_d = nc.dram_tensor("wsm", [128, 576], F32, kind="ExternalInput").ap()
    cst_d = nc.dram_tensor("cst", [128, NCST], F32, kind="ExternalInput").ap()
    out_d = nc.dram_tensor("out", [ntok, D], F32, kind="ExternalOutput").ap()
    scr_d = nc.dram_tensor("scr", [NPIECE, 128, 4096], BF16).ap()
    dbg_outs = {}

    P = Prog()
    es = ExitStack()
    with es:
        def sb(name, shape, dt):
            return es.enter_context(nc.sbuf_tensor(name, shape, dt))

        stage_in = sb("stage_in", [128, NSUB, D], F32)
        stage_out = sb("stage_out", [128, 4, D], F32)
        xT = sb("xT", [128, KC, T], F32)
        xn = sb("xn", [128, KC, T], BF16)
        rs = sb("rs", [128, 2, T], F32)
        hid = sb("hid", [128, NFC, T], BF16)
        tmpf = sb("tmpf", [128, 2, T], F32)
        tmpb = sb("tmpb", [128, 2, T], BF16)
        zqk = sb("zqk", [128, KC, 516], BF16)
        qkT = sb("qkT", [128, KC, T], BF16)
        ktm = sb("ktm", [128, NSUB, T], BF16)
        vaug = sb("vaug", [128, NSUB, H, 130], BF16)
        U = sb("U", [128, 5, T], BF16)
        poolT = sb("poolT", [128, 4, T], BF16)
        ybT = sb("ybT", [128, 4, T], BF16)
        spT = sb("spT", [128, 2, T], BF16)
        ytm = sb("ytm", [128, 2, T], BF16)
        yaT = sb("yaT", [128, H, T], BF16)
        Cf = sb("Cf", [128, H, 130], F32)
        Cbf = sb("Cbf", [128, H, 130], BF16)
        zif = sb("zif", [128, NSUB, 8], F32)
        ef = sb("ef", [128, NSUB, 4], F32)
        nlf = sb("nlf", [128, NSUB, 4], F32)
        eb = sb("eb", [128, NSUB, 4], F32)
        eB = sb("eB", [128, NSUB, 4], F32)
        inv2 = sb("inv2", [128, NSUB, 4], F32)
        tmpi = sb("tmpi", [128, NSUB, 4], F32)
        wgt = sb("wgt", [128, NSUB, 4], F32)
        sc = sb("sc", [128, 10, 4], F32)
        ss = sb("ss", [128, NSUB, 4], F32)
        frs = sb("frs", [128, 2, 4], F32)
        junk = sb("junk", [128, 128], BF16)
        dmy = sb("dmy", [128, 4], F32)
        cbf = sb("cbf", [128, NCST], BF16)
        c32 = sb("c32", [128, 384], F32)
        diag = sb("diag", [128, 32, 128], BF16)
        wsm = sb("wsm_s", [128, 576], BF16)
        pv = sb("pv_s", [128, NPV], F32)
        bif = sb("bif", [128, 8], F32)
        bv = sb("bv", [1, 512], BF16)
        gfin = sb("gfin", [128, D], F32)
        wslot = [sb(f"wslot{i}", [128, 4096], BF16) for i in range(NSLOT)]
        ps = es.enter_context(nc.psum_tensor("ps", [128, 4096], F32))
        psb = ps[:, :].bitcast(BF16)

        def PS(b, lo=0, hi=512):
            return ps[:, b * 512 + lo: b * 512 + hi]

        def PSB(b, lo, hi):
            return psb[:, b * 1024 + lo: b * 1024 + hi]

        ident32 = c32[:, C_ID:C_ID + 128]
        tri32 = c32[:, C_TRI:C_TRI + 128]
        ones32 = c32[:, C_ONE:C_ONE + 128]
        identb = cbf[:, C_ID:C_ID + 128]
        trib = cbf[:, C_TRI:C_TRI + 128]
        onesb = cbf[:, C_ONE:C_ONE + 128]

        def band(kind, g):
            o = C_BAND + (kind * 4 + g) * 128
            return cbf[:, o:o + 128]

        def pcol(c, n=1):
            return pv[:, c:c + n]

        def mm(out, lhsT, rhs, start, stop):
            P.op("pe", lambda e: e.matmul(out, lhsT, rhs, start=start, stop=stop),
                 reads=[lhsT, rhs], writes=[out])

        def tr(out, in_, ident):
            P.op("pe", lambda e: e.transpose(out, in_, ident), reads=[in_, ident], writes=[out])

        def act(out, in_, func, bias=None, scale=None, accum=None):
            kw = {}
            rd = [in_]
            wr = [out]
            if bias is not None:
                kw["bias"] = bias
                if not isinstance(bias, float):
                    rd.append(bias)
            if scale is not None:
                kw["scale"] = scale
                if not isinstance(scale, float):
                    rd.append(scale)
            if accum is not None:
                kw["accum_out"] = accum
                wr.append(accum)
            P.op("act", lambda e: e.activation(out, in_, func, **kw), reads=rd, writes=wr)

        def tt(out, in0, in1, op, eng="dve"):
            P.op(eng, lambda e: e.tensor_tensor(out, in0, in1, op), reads=[in0, in1], writes=[out])

        def stt(out, in0, scalar, in1, op0, op1):
            rd = [in0, in1] + ([] if isinstance(scalar, float) else [scalar])
            P.op("dve", lambda e: e.scalar_tensor_tensor(out, in0, scalar, in1, op0, op1),
                 reads=rd, writes=[out])

        def ts(out, in0, s1, op0, s2=None, op1=None):
            rd = [in0] + ([] if isinstance(s1, float) else [s1])
            if op1 is None:
                P.op("dve", lambda e: e.tensor_single_scalar(out, in0, s1, op0), reads=rd, writes=[out])
            else:
                P.op("dve", lambda e: e.tensor_scalar(out, in0, s1, s2, op0, op1), reads=rd, writes=[out])

        def cp(out, in_, eng="dve"):
            if eng == "act":
                P.op("act", lambda e: e.copy(out, in_), reads=[in_], writes=[out])
            else:
                P.op(eng, lambda e: e.tensor_copy(out, in_), reads=[in_], writes=[out])

        def memset(ap, v, eng="dve"):
            P.op(eng, lambda e: e.memset(ap, v), writes=[ap])

        def dma(eng, chan, out, in_, reads=(), writes=()):
            P.op(eng, lambda e: e.dma_start(out=out, in_=in_), reads=list(reads), writes=list(writes), chan=chan)

        def dump(name, ap, shape, dt=F32):
            if dbg is None or name not in dbg:
                return
            d = nc.dram_tensor("dbg_" + name, list(shape), dt, kind="ExternalOutput").ap()
            dbg_outs[name] = d
            dma("pool", P.dma_chan("dbg_" + name), d, ap, reads=[ap])

        ch_xin = P.dma_chan("xin")
        ch_out = [P.dma_chan(f"out{q}") for q in range(4)]
        ch_w = [P.dma_chan(f"w{i}") for i in range(NSLOT)]
        ch_st = [P.dma_chan(f"st{i}") for i in range(NSLOT)]
        ch_wc = [P.dma_chan(f"wc{i}") for i in range(NSLOT)]

        def load_x(i):
            src = x_d[i * T:(i + 1) * T, :].rearrange("(j p) d -> p j d", p=128)
            dma("pool", ch_xin, stage_in[:, :, :], src, writes=[stage_in[:, :, :]])

        for nm, dst, src in (
            ("pv", pv[:, :], pv_d),
            ("c32", c32[:, :], cst_d[:, 0:384]),
            ("cbf", cbf[:, :], cst_d),
            ("wsm", wsm[:, :], wsm_d),
            ("bif", bif[:, :], rowp_d[:, 0:8].partition_broadcast(128)),
            ("bv", bv[:, :], rowp_d[:, 8:520]),
            ("gfin", gfin[:, :], rowp_d[:, 520:1544].partition_broadcast(128)),
        ):
            dma("pool", P.dma_chan("su_" + nm), dst, src, writes=[dst])
        load_x(0)
        memset(dmy[:, :], 1.0)
        memset(Cf[:, :, :], 0.0)
        memset(Cbf[:, :, :], 0.0)
        memset(zqk[:, :, 0:3], 0.0)
        memset(U[:, 0, :], 0.0)
        memset(vaug[:, :, :, :], 0.0)
        for j in range(4):
            for ch in range(KC):
                ts(diag[:, j * 8 + ch, :], identb, pcol(CONV + j * 8 + ch), ALU.mult)

        wstate = {"n": 0}

        converted = set()

        def load_piece(k):
            s = wstate["n"] % NSLOT
            wstate["n"] += 1
            nco = 2816 if (11 <= k < 19 or 43 <= k < 51) else 4096
            if k not in converted:
                converted.add(k)
                dma("pool", ch_wc[s], wslot[s][:, 0:nco], wsrc_d[k][:, 0:nco], writes=[wslot[s][:, :]])
                dma("sp", ch_st[s], scr_d[k][:, 0:nco], wslot[s][:, 0:nco], reads=[wslot[s][:, :]], writes=[("scr", k, k + 1)])
            else:
                dma("sp", ch_w[s], wslot[s][:, 0:nco], scr_d[k][:, 0:nco], reads=[("scr", k, k + 1)], writes=[wslot[s][:, :]])
            return wslot[s]

        def preload_ln():
            act(dmy[:, 2:3], dmy[:, 0:1], AF.Ln)

        NBANK = 7

        sq_pending = []

        def sq_flush():
            while sq_pending:
                k, buf = sq_pending.pop(0)
                mm(PS(NBANK), onesb, buf[:, k, :], k == 0, k == KC - 1)

        def sq_chunk(k, buf):
            act(buf[:, k, :], xT[:, k, :], AF.Square)
            sq_flush()
            sq_pending.append((k, buf))

        def norm_finish(gcol):
            sq_flush()
            act(rs[:, 0, :], PS(NBANK), AF.Ln, bias=EPS, scale=1.0 / D)
            act(rs[:, 1, :], rs[:, 0, :], AF.Exp, scale=-0.5)
            for k in range(KC):
                stt(xn[:, k, :], xT[:, k, :], pcol(gcol + k), rs[:, 1, :], ALU.mult, ALU.mult)

        def ffn(piece0, gcol, after_chunk):
            norm_finish(gcol)
            w = load_piece(piece0)
            fb = [P.bank() for _ in range(4)]
            for k in range(KC):
                for gi in range(4):
                    col = k * 512 + (gi % 2) * 256 + (gi // 2) * 128
                    mm(PS(fb[gi]), w[:, col:col + 128], xn[:, k, :], k == 0, k == KC - 1)
            for jj in range(2):
                act(tmpf[:, jj % 2, :], PS(fb[2 * jj]), AF.Silu)
                tt(hid[:, jj, :], tmpf[:, jj % 2, :], PS(fb[2 * jj + 1]), ALU.mult)
            for p in range(1, 11):
                w = load_piece(piece0 + p)
                for jj in range(2):
                    j = 2 * p + jj
                    bg = P.bank()
                    for k in range(KC):
                        mm(PS(bg), w[:, k * 512 + jj * 128: k * 512 + jj * 128 + 128], xn[:, k, :], k == 0, k == KC - 1)
                    bu = P.bank()
                    for k in range(KC):
                        mm(PS(bu), w[:, k * 512 + 256 + jj * 128: k * 512 + 256 + jj * 128 + 128], xn[:, k, :], k == 0, k == KC - 1)
                    act(tmpf[:, j % 2, :], PS(bg), AF.Silu)
                    tt(hid[:, j, :], tmpf[:, j % 2, :], PS(bu), ALU.mult)
            preload_ln()
            for j in range(KC):
                w = load_piece(piece0 + 11 + j)
                b = P.bank()
                for k in range(NFC):
                    mm(PS(b), w[:, k * 128:(k + 1) * 128], hid[:, k, :], k == 0, k == NFC - 1)
                stt(xT[:, j, :], PS(b), 0.5, xT[:, j, :], ALU.mult, ALU.add)
                after_chunk(j)

        def proj_fm(w, cc, evac, bank=None):
            b = P.bank() if bank is None else bank
            for k in range(KC):
                mm(PS(b), w[:, k * 512 + cc * 128: k * 512 + cc * 128 + 128], xn[:, k, :], k == 0, k == KC - 1)
            evac(b)

        def mixer(i):
            norm_finish(GM)
            bgt = P.bank()
            for j in range(NSUB):
                for k in range(KC):
                    mm(PS(bgt, j * 8, j * 8 + 8), xn[:, k, j * 128:(j + 1) * 128], wsm[:, k * 8:(k + 1) * 8], k == 0, k == KC - 1)
            tt(zif[:, :, :], PS(bgt, 0, 32).rearrange("p (j c) -> p j c", j=NSUB),
               bif[:, :].unsqueeze(1).to_broadcast([128, NSUB, 8]), ALU.add)
            act(ef[:, :, :], zif[:, :, 4:8], AF.Exp, scale=-1.0)
            act(nlf[:, :, :], ef[:, :, :], AF.Ln, bias=1.0)
            bcs = P.bank()
            for j in range(NSUB):
                mm(PS(bcs, j * 4, j * 4 + 4), tri32, nlf[:, j, :], True, True)
            for j in range(NSUB):
                mm(PS(bcs, 16 + j * 4, 16 + j * 4 + 4), ones32, nlf[:, j, :], True, True)
            act(eb[:, :, :], PS(bcs, 0, 16).rearrange("p (j c) -> p j c", j=NSUB), AF.Exp, scale=-1.0)
            act(eB[:, :, :], PS(bcs, 16, 32).rearrange("p (j c) -> p j c", j=NSUB), AF.Exp, scale=-1.0)
            act(inv2[:, :, :], PS(bcs, 0, 16).rearrange("p (j c) -> p j c", j=NSUB), AF.Exp, scale=2.0)
            tt(tmpi[:, :, :], zif[:, :, 0:4], PS(bcs, 0, 16).rearrange("p (j c) -> p j c", j=NSUB), ALU.add)
            act(wgt[:, :, :], tmpi[:, :, :], AF.Exp)
            cp(vaug[:, :, :, 128:129], wgt[:, :, :].unsqueeze(3))
            if stop == "mx_gates":
                return True
            for pc in range(2):
                w = load_piece(19 + pc)
                for cc in range(4):
                    ch = pc * 4 + cc
                    proj_fm(w, cc, lambda b, ch=ch: ts(zqk[:, ch, 3:515], PS(b), pcol(BQK + ch), ALU.add))
            if stop == "mx_qk":
                return True
            for ch in range(KC):
                b = P.bank()
                for j in range(4):
                    mm(PS(b), diag[:, j * 8 + ch, :], zqk[:, ch, j:j + 512], j == 0, j == 3)
                act(qkT[:, ch, :], PS(b), AF.Silu)
            cp(zqk[:, :, 0:3], zqk[:, :, 512:515])
            if stop == "mx_conv":
                return True
            for j in range(NSUB):
                b = P.bank()
                for h in range(H):
                    tr(PSB(b, h * 128, (h + 1) * 128), qkT[:, 4 + h, j * 128:(j + 1) * 128], identb)
                cp(ktm[:, j, :], PSB(b, 0, 512), eng="act")
            if stop == "mx_ktr":
                return True
            w = load_piece(21)
            for j in range(NSUB):
                b = P.bank()
                for k in range(KC):
                    mm(PS(b), xn[:, k, j * 128:(j + 1) * 128], w[:, k * 512:(k + 1) * 512], k == 0, False)
                mm(PS(b), cbf[0:1, C_ONE:C_ONE + 128], bv[0:1, :], False, True)
                tt(vaug[:, j, :, 0:128], PS(b).rearrange("p (h v) -> p h v", h=H),
                   wgt[:, j, :].unsqueeze(2).to_broadcast([128, H, 128]), ALU.mult)
            if stop == "mx_v":
                return True
            w = load_piece(22)
            for cc in range(4):
                proj_fm(w, cc, lambda b, cc=cc: act(hid[:, 16 + cc, :], PS(b), AF.Sigmoid, bias=pcol(BO + cc)))
            if stop == "mx_o":
                return True
            preload_ln()
            if i > 0:
                cp(U[:, 0, :], U[:, 4, :])
            w = load_piece(23)
            for j in range(NSUB):
                b = P.bank()
                for k in range(KC):
                    mm(PS(b), xn[:, k, j * 128:(j + 1) * 128], w[:, k * 512:(k + 1) * 512], k == 0, k == KC - 1)
                cp(U[:, 1 + j, :], PS(b), eng="act")
            for g in range(4):
                b = P.bank()
                for j in range(NSUB):
                    first = first_tile_global and i == 0 and j == 0
                    mm(PS(b, j * 128, (j + 1) * 128), U[:, j, g * 128:(g + 1) * 128], band(1, g), True, False)
                    mm(PS(b, j * 128, (j + 1) * 128), U[:, j + 1, g * 128:(g + 1) * 128], band(2 if first else 0, g), False, True)
                cp(poolT[:, g, :], PS(b))
            for g in range(4):
                b = P.bank()
                mm(PS(b), wsm[:, 64 + g * 128: 64 + (g + 1) * 128], poolT[:, g, :], True, True)
                act(ybT[:, g, :], PS(b), AF.Copy, scale=pcol(PSC + g))
            if stop == "mx_pool":
                return True
            gate_w = {}

            def gate_filler(which, c):
                def run(bank=None):
                    pidx = (24 if which == 0 else 26) + c // 4
                    if pidx not in gate_w:
                        gate_w[pidx] = load_piece(pidx)
                    bcol = (BGA if which == 0 else BGB) + c
                    dst = hid[:, (0 if which == 0 else 8) + c, :]
                    proj_fm(gate_w[pidx], c % 4, lambda b: act(dst, PS(b), AF.Identity, bias=pcol(bcol)), bank=bank)
                return run

            fillers = [gate_filler(wh, c) for wh in range(2) for c in range(KC)]

            def fill():
                if fillers:
                    fillers.pop(0)()

            memset(ss[:, :, :], 0.0)
            small = {"n": 0}

            def small_bank():
                small["n"] += 1
                return 6 + small["n"] % 2

            ACCB = (0, 2)
            CBANK = 4
            S_ = lambda n: sc[:, n, :]

            def st_S(j):
                tok = slice(j * 128, (j + 1) * 128)
                bS = small_bank()
                for h in range(H):
                    mm(PS(bS, h * 128, (h + 1) * 128), qkT[:, 4 + h, tok], qkT[:, h, tok], True, True)
                tt(spT[:, j % 2, :].rearrange("p (h t) -> p h t", h=H), PS(bS).rearrange("p (h t) -> p h t", h=H),
                   trib.unsqueeze(1).to_broadcast([128, H, 128]), ALU.mult)

            def st_A(j):
                tok = slice(j * 128, (j + 1) * 128)
                par = j % 2
                bA = ACCB[par]
                for h in range(H):
                    o_ = ps[:, bA * 512 + h * 256: bA * 512 + h * 256 + 130]
                    mm(o_, spT[:, par, h * 128:(h + 1) * 128], vaug[:, j, h, 0:130], True, False)
                    mm(o_, qkT[:, h, tok], Cbf[:, h, 0:130], False, True)
                acc = ps[:, bA * 512: bA * 512 + 1024].rearrange("p (h c) -> p h c", h=H)
                for h in range(H):
                    act(junk[:, :], acc[:, h, 0:128], AF.Square, accum=ss[:, j, h:h + 1])
                bC = CBANK
                for h in range(H):
                    mm(ps[:, bC * 512 + h * 256: bC * 512 + h * 256 + 130], ktm[:, j, h * 128:(h + 1) * 128], vaug[:, j, h, 0:130], True, True)
                accC = ps[:, bC * 512: bC * 512 + 1024].rearrange("p (h c) -> p h c", h=H)
                tt(Cf[:, :, :], Cf[:, :, :], accC[:, :, 0:130], ALU.add)
                tt(Cf[:, :, :], Cf[:, :, :], eB[:, j, :].unsqueeze(2).to_broadcast([128, H, 130]), ALU.mult)
                cp(Cbf[:, :, :], Cf[:, :, :])

            def st_scal(j):
                par = j % 2
                bA = ACCB[par]
                acc = ps[:, bA * 512: bA * 512 + 1024].rearrange("p (h c) -> p h c", h=H)
                stt(S_(0).unsqueeze(2), acc[:, :, 128:129], SCALE, eb[:, j, :].unsqueeze(2), ALU.mult, ALU.mult)
                tt(S_(1), S_(0), S_(0), ALU.mult)
                ts(S_(2), S_(1), 1.0, ALU.max, EPS / (SCALE * SCALE), ALU.mult)
                tt(S_(3), S_(2), inv2[:, j, :], ALU.mult)
                stt(S_(4), ss[:, j, :], 1.0 / 128.0, S_(3), ALU.mult, ALU.add)
                act(S_(5), S_(4), AF.Ln)
                act(S_(6), S_(5), AF.Exp, scale=-0.5)
                tt(ytm[:, par, :].rearrange("p (h v) -> p h v", h=H), acc[:, :, 0:128],
                   S_(6).unsqueeze(2).to_broadcast([128, H, 128]), ALU.mult)

            def st_T(j):
                tok = slice(j * 128, (j + 1) * 128)
                par = j % 2
                bT = small_bank()
                for h in range(H):
                    tr(PSB(bT, h * 128, (h + 1) * 128), ytm[:, par, h * 128:(h + 1) * 128], identb)
                for h in range(H):
                    stt(yaT[:, h, tok], PSB(bT, h * 128, (h + 1) * 128), pcol(HNG + h), hid[:, 16 + h, tok], ALU.mult, ALU.mult)

            def fillS():
                if fillers:
                    fillers.pop(0)(small_bank())

            st_S(0)
            fillS()
            st_A(0)
            fillS()
            for j in range(NSUB):
                if j + 1 < NSUB:
                    st_S(j + 1)
                    fillS()
                st_scal(j)
                if j + 1 < NSUB:
                    st_A(j + 1)
                    fillS()
                if j >= 1:
                    st_T(j - 1)
                    fillS()
            st_T(NSUB - 1)
            if stop == "mx_lstm":
                return True
            while fillers:
                fillers.pop(0)()
            for c in range(KC):
                act(hid[:, c, :], hid[:, c, :], AF.Sigmoid)
                act(hid[:, 8 + c, :], hid[:, 8 + c, :], AF.Sigmoid)
            preload_ln()
            wa = load_piece(28)
            wb = load_piece(29)
            for c in range(KC):
                ba = P.bank()
                for k in range(4):
                    mm(PS(ba), wa[:, k * 1024 + c * 128: k * 1024 + c * 128 + 128], yaT[:, k, :], k == 0, k == 3)
                bb = P.bank()
                for k in range(4):
                    mm(PS(bb), wb[:, k * 1024 + c * 128: k * 1024 + c * 128 + 128], ybT[:, k, :], k == 0, k == 3)
                tt(tmpb[:, 0, :], PS(ba), hid[:, c, :], ALU.mult)
                tt(tmpb[:, 1, :], PS(bb), hid[:, 8 + c, :], ALU.mult)
                tt(xn[:, c, :], tmpb[:, 0, :], tmpb[:, 1, :], ALU.add)
            if stop == "mx_merge":
                return True
            for pc in range(2):
                w = load_piece(30 + pc)
                for cc in range(4):
                    c = pc * 4 + cc
                    proj_fm(w, cc, lambda b, c=c: tt(xT[:, c, :], PS(b), xT[:, c, :], ALU.add))
                    sq_chunk(c, qkT)

        for i in range(nt if stop != "setup" else 0):
            if i > 0:
                P.new_epoch()
            for k in range(KC):
                b = (6, 2, 3, 4, 5, 6, 2, 3)[k]
                for j in range(NSUB):
                    tr(PS(b, j * 128, (j + 1) * 128), stage_in[:, j, k * 128:(k + 1) * 128], ident32)
                cp(xT[:, k, :], PS(b), eng=("act" if (k < 4 or k % 2) else "dve"))
                sq_chunk(k, xn)
            if i + 1 < nt:
                load_x(i + 1)
            if i == 0:
                dump("x0", xT[:, :, :], [128, KC, T])
            if stop == "input":
                break
            ffn(0, G1, lambda j: sq_chunk(j, xn))
            if i == 0:
                dump("x1", xT[:, :, :], [128, KC, T])
            if stop == "ffn1":
                break
            if mixer(i):
                break
            if i == 0:
                dump("x2", xT[:, :, :], [128, KC, T])
                dump("qkT", qkT[:, :, :], [128, KC, T], BF16)
                dump("yaT", yaT[:, :, :], [128, H, T], BF16)
                dump("ybT", ybT[:, :, :], [128, 4, T], BF16)
                dump("vaug", vaug[:, :, :, :], [128, NSUB, H, 130], BF16)
                dump("eb", eb[:, :, :], [128, NSUB, 4])
                dump("wgt", wgt[:, :, :], [128, NSUB, 4])
                dump("Cf", Cf[:, :, :], [128, H, 130])
            if stop == "mixer":
                break
            ffn(32, G2, lambda j: act(xn[:, j, :], xT[:, j, :], AF.Square))
            if i == 0:
                dump("x3", xT[:, :, :], [128, KC, T])
            if stop == "ffn2":
                break
            bf = 6
            for j in range(NSUB):
                for k in range(KC):
                    mm(PS(bf, j, j + 1), xn[:, k, j * 128:(j + 1) * 128], onesb[:, 0:1], k == 0, k == KC - 1)
            act(frs[:, 0, :], PS(bf, 0, 4), AF.Ln, bias=EPS, scale=1.0 / D)
            act(frs[:, 1, :], frs[:, 0, :], AF.Exp, scale=-0.5)
            for j in range(NSUB):
                b2 = (0, 2, 4, 0)[j]
                for k in range(KC):
                    tr(ps[:, b2 * 512 + k * 128: b2 * 512 + (k + 1) * 128], xT[:, k, j * 128:(j + 1) * 128], ident32)
                so = stage_out[:, j, :]
                stt(so, ps[:, b2 * 512: b2 * 512 + 1024], frs[:, 1, j:j + 1], gfin[:, :], ALU.mult, ALU.mult)
                r0 = i * T + j * 128
                dma("pool", ch_out[j], out_d[r0:r0 + 128, :], so, reads=[so])

        P.finalize(nc, es)
    return nc, dbg_outs


def _kmaj(W, c0, n):
    K = W.shape[0]
    return W[:, c0:c0 + n].reshape(K // 128, 128, n).transpose(1, 0, 2)


def _pad(a):
    a = a.reshape(128, -1)
    out = np.zeros((128, 4096), np.float32)
    out[:, :a.shape[1]] = a
    return out


def _consts():
    c = np.zeros((128, NCST), np.float32)
    s = np.arange(128)[:, None]
    t = np.arange(128)[None, :]
    c[:, C_ID:C_ID + 128] = (s == t)
    c[:, C_TRI:C_TRI + 128] = (s <= t)
    c[:, C_ONE:C_ONE + 128] = 1.0
    for g, w in enumerate((2, 4, 8, 16)):
        inwin = ((t - s) >= 0) & ((t - s) <= w - 1)
        cur = inwin / w - (s == t)
        prev = ((t - s + 128) <= w - 1) / w
        cnt = np.minimum(t + 1, w)
        first = inwin / cnt - (s == t)
        c[:, C_BAND + (0 * 4 + g) * 128: C_BAND + (0 * 4 + g + 1) * 128] = cur
        c[:, C_BAND + (1 * 4 + g) * 128: C_BAND + (1 * 4 + g + 1) * 128] = prev
        c[:, C_BAND + (2 * 4 + g) * 128: C_BAND + (2 * 4 + g + 1) * 128] = first
    return c


def _layout(inp):
    f = lambda a: np.asarray(a, dtype=np.float32)
    pieces = np.zeros((NPIECE, 128, 4096), np.float32)

    def ffn_pieces(base, wgu, wd):
        for p in range(11):
            a = np.concatenate([_kmaj(wgu, 256 * p, 256), _kmaj(wgu, DFF + 256 * p, 256)], axis=2)
            pieces[base + p] = a.reshape(128, 4096)
        for j in range(8):
            pieces[base + 11 + j] = _pad(_kmaj(wd, 128 * j, 128))

    ffn_pieces(0, f(inp["ffn1_w_gu"])[0], f(inp["ffn1_w_down"])[0])
    ffn_pieces(32, f(inp["ffn2_w_gu"])[0], f(inp["ffn2_w_down"])[0])
    win = f(inp["w_in"])[0]
    for n, c0 in ((19, 0), (20, 512), (21, 1024), (22, 1536), (23, 2048), (24, 2560), (25, 3072), (26, 3584), (27, 4096)):
        pieces[n] = _kmaj(win, c0, 512).reshape(128, 4096)
    pieces[28] = _kmaj(f(inp["p_a"])[0], 0, 1024).reshape(128, 4096)
    pieces[29] = _kmaj(f(inp["p_b"])[0], 0, 1024).reshape(128, 4096)
    wo = f(inp["w_out"])[0]
    pieces[30] = _kmaj(wo, 0, 512).reshape(128, 4096)
    pieces[31] = _kmaj(wo, 512, 512).reshape(128, 4096)

    col = lambda v: f(v).reshape(-1, 128).T
    pvv = np.zeros((128, NPV), np.float32)
    b_in = f(inp["b_in"])[0]
    pvv[:, G1:G1 + 8] = col(inp["ffn1_norm_g"][0])
    pvv[:, GM:GM + 8] = col(inp["mix_norm_g"][0])
    pvv[:, G2:G2 + 8] = col(inp["ffn2_norm_g"][0])
    pvv[:, BQK:BQK + 8] = col(b_in[0:1024])
    pvv[:, BO:BO + 4] = col(b_in[1536:2048])
    pvv[:, BGA:BGA + 8] = col(b_in[2560:3584])
    pvv[:, BGB:BGB + 8] = col(b_in[3584:4608])
    pvv[:, HNG:HNG + 4] = col(inp["head_norm_g"][0])
    pvv[:, PSC:PSC + 4] = col(inp["pool_scale"][0])
    cw = f(inp["conv_qk"])[0]
    for j in range(4):
        pvv[:, CONV + j * 8: CONV + j * 8 + 8] = col(cw[j])
    rowp = np.concatenate([b_in[4608:4616], b_in[1024:1536], f(inp["final_norm_g"])]).reshape(1, -1)
    wsmv = np.zeros((128, 576), np.float32)
    wsmv[:, 0:64] = _kmaj(win, 4608, 8).reshape(128, 64)
    wsmv[:, 64:576] = f(inp["w_pool"])[0].transpose(1, 0, 2).reshape(128, 512)
    return {"wsrc": pieces, "pv": pvv, "rowp": np.ascontiguousarray(rowp), "wsm": wsmv, "cst": _consts()}


_NC_CACHE = {}


def kernel(**inputs):
    x = np.asarray(inputs["x"], dtype=np.float32)
    shared = _layout(inputs)
    nt = SEQ // T
    if nt not in _NC_CACHE:
        _NC_CACHE[nt] = build_nc(nt)[0]
    nc = _NC_CACHE[nt]
    in_maps = [dict(shared, x=np.ascontiguousarray(x[b])) for b in range(NB)]
    res = run_bass_kernel_spmd(nc, in_maps, core_ids=list(range(NB)))
    return np.stack([np.asarray(r["out"]) for r in res.results], axis=0).astype(np.float32)
```

```python
import numpy as np
from contextlib import ExitStack

import concourse.bass as bass
import concourse.mybir as mybir
from concourse.bass_utils import run_bass_kernel_spmd

F32 = mybir.dt.float32
BF16 = mybir.dt.bfloat16
AF = mybir.ActivationFunctionType
ALU = mybir.AluOpType

D = 1024
SEQ = 8192
NB = 8
T = 512
NSUB = 4
KC = 8
DFF = 2816
NFC = 22
H = 4
EPS = 1e-6
SCALE = 128.0 ** -0.5
NPIECE = 51
NSLOT = 6
NCONV = 8

G1, GM, G2, BQK, BO, BGA, BGB, HNG, PSC, CONV = 0, 8, 16, 24, 32, 36, 44, 52, 56, 60
NPV = 92
C_ID, C_TRI, C_ONE, C_BAND = 0, 128, 256, 384
NCST = 384 + 12 * 128


def _esz(dt):
    return 2 if dt == BF16 else 4


class Chan:
    def __init__(self, name, is_dma, eng=None):
        self.name = name
        self.is_dma = is_dma
        self.eng = eng
        self.ops = []
        self.sem = None


class Op:
    __slots__ = ("eng", "chan", "fn", "deps", "needs_inc", "idx", "value")


class Rec:
    __slots__ = ("lo", "hi", "writers", "readers", "ov")


class Prog:
    ENGS = ("pe", "act", "dve", "pool", "sp")

    def __init__(self):
        self.eng_ops = {e: [] for e in self.ENGS}
        self.dma_chans = []
        self.all_eng_chans = []
        self.epoch = -1
        self.new_epoch()
        self.state = {}
        self._bank = 0

    def new_epoch(self):
        self.epoch += 1
        self.eng_chan = {e: Chan(f"{e}{self.epoch}", False, e) for e in ("pe", "act", "dve", "pool")}
        self.all_eng_chans += list(self.eng_chan.values())

    def dma_chan(self, name):
        c = Chan(name, True)
        self.dma_chans.append(c)
        return c

    def _key(self, a):
        if isinstance(a, tuple):
            return a
        dims = a.ap
        pstep = dims[0][0]
        start = a.offset % pstep if pstep > 0 else a.offset
        lo = hi = start
        for step, cnt in dims[1:]:
            if step >= 0:
                hi += step * (cnt - 1)
            else:
                lo += step * (cnt - 1)
        e = _esz(a.dtype)
        lo, hi = lo * e, (hi + 1) * e
        if a.tensor.name == "ps":
            lo = (lo // 2048) * 2048
            hi = ((hi + 2047) // 2048) * 2048
        return (a.tensor.name, lo, hi)

    def _rec(self, key):
        name, lo, hi = key
        recs = self.state.setdefault(name, {})
        r = recs.get((lo, hi))
        if r is None:
            r = Rec()
            r.lo, r.hi, r.writers, r.readers, r.ov = lo, hi, {}, {}, []
            for o in recs.values():
                if o.lo < hi and lo < o.hi:
                    o.ov.append(r)
                    r.ov.append(o)
            r.ov.append(r)
            recs[(lo, hi)] = r
        return r

    def op(self, eng, fn, reads=(), writes=(), chan=None):
        o = Op()
        o.eng, o.fn, o.needs_inc, o.value = eng, fn, False, None
        o.chan = chan if chan is not None else self.eng_chan[eng]
        o.idx = len(o.chan.ops)
        deps = {}

        def add(d):
            cur = deps.get(d.chan)
            if cur is None or d.idx > cur.idx:
                deps[d.chan] = d

        if o.chan.is_dma and o.idx > 0:
            add(o.chan.ops[o.idx - 1])
        rkeys = [self._key(a) for a in reads]
        wkeys = [self._key(a) for a in writes]
        psk = [k for k in rkeys + wkeys if k[0] == "ps"]
        rkeys = [k for k in rkeys if k[0] != "ps"]
        wkeys = [k for k in wkeys if k[0] != "ps"]
        for k in psk:
            for b in range(k[1] // 2048, k[2] // 2048):
                kb = ("ps", b * 2048, (b + 1) * 2048)
                if kb not in wkeys:
                    wkeys.append(kb)
        rrecs = [self._rec(k) for k in rkeys]
        wrecs = [self._rec(k) for k in wkeys]
        for r in rrecs:
            for x in r.ov:
                for w in x.writers.values():
                    add(w)
        for r in wrecs:
            for x in r.ov:
                for w in x.writers.values():
                    add(w)
                for w in x.readers.values():
                    add(w)
        for r in rrecs:
            r.readers[o.chan] = o
        for r in wrecs:
            for x in r.ov:
                if x is r:
                    x.writers = {o.chan: o}
                    x.readers = {}
                else:
                    x.writers[o.chan] = o
        if eng == "pe":
            for ch in [c for c in deps if c.eng == "pe"]:
                del deps[ch]
        o.deps = deps
        o.chan.ops.append(o)
        self.eng_ops[eng].append(o)
        return o

    def bank(self):
        b = self._bank
        self._bank = (b + 1) % 7
        return b

    def bank2(self):
        b = self._bank
        while b % 2 or b >= 6:
            b = (b + 1) % 7
        self._bank = (b + 2) % 7
        return b

    def finalize(self, nc, es):
        for ops in self.eng_ops.values():
            for o in ops:
                for d in o.deps.values():
                    d.needs_inc = True
        for c in [c for c in self.all_eng_chans if c.ops] + self.dma_chans:
            c.sem = es.enter_context(nc.semaphore(c.name))
            n = 0
            for o in c.ops:
                if c.is_dma:
                    n += 16
                    o.needs_inc = True
                elif o.needs_inc:
                    n += 1
                o.value = n
        block = es.enter_context(nc.Block())
        names = {"pe": "tensor", "act": "scalar", "dve": "vector", "pool": "gpsimd", "sp": "sync"}

        def make(eng):
            ops = self.eng_ops[eng]

            def body(e):
                waited = {}
                last_dma = {}
                for o in ops:
                    for ch, d in o.deps.items():
                        if waited.get(ch, 0) < d.value:
                            e.wait_ge(ch.sem, d.value)
                            waited[ch] = d.value
                    ins = o.fn(e)
                    if o.needs_inc:
                        ins.then_inc(o.chan.sem, 16 if o.chan.is_dma else 1)
                    if o.chan.is_dma:
                        last_dma[o.chan] = o.value
                for ch, v in last_dma.items():
                    if waited.get(ch, 0) < v:
                        e.wait_ge(ch.sem, v)
            return body

        for eng in self.ENGS:
            if self.eng_ops[eng]:
                getattr(block, names[eng])(make(eng))


def build_nc(nt, first_tile_global=True, dbg=None, stop=None):
    nc = bass.Bass("TRN2", target_bir_lowering=False)
    ntok = nt * T
    x_d = nc.dram_tensor("x", [ntok, D], F32, kind="ExternalInput").ap()
    wsrc_d = nc.dram_tensor("wsrc", [NPIECE, 128, 4096], F32, kind="ExternalInput").ap()
    pv_d = nc.dram_tensor("pv", [128, NPV], F32, kind="ExternalInput").ap()
    rowp_d = nc.dram_tensor("rowp", [1, 8 + 512 + 1024], F32, kind="ExternalInput").ap()
    wsm_d = nc.dram_tensor("wsm", [128, 576], F32, kind="ExternalInput").ap()
    cst_d = nc.dram_tensor("cst", [128, NCST], F32, kind="ExternalInput").ap()
    out_d = nc.dram_tensor("out", [ntok, D], F32, kind="ExternalOutput").ap()
    scr_d = nc.dram_tensor("scr", [NPIECE, 128, 4096], BF16).ap()
    dbg_outs = {}

    P = Prog()
    es = ExitStack()
    with es:
        def sb(name, shape, dt):
            return es.enter_context(nc.sbuf_tensor(name, shape, dt))

        stage_in = sb("stage_in", [128, NSUB, D], F32)
        stage_out = sb("stage_out", [128, 4, D], F32)
        xT = sb("xT", [128, KC, T], F32)
        xn = sb("xn", [128, KC, T], BF16)
        rs = sb("rs", [128, 2, T], F32)
        hid = sb("hid", [128, NFC, T], BF16)
        tmpf = sb("tmpf", [128, 2, T], F32)
        tmpb = sb("tmpb", [128, 2, T], BF16)
        zqk = sb("zqk", [128, KC, 516], BF16)
        qkT = sb("qkT", [128, KC, T], BF16)
        ktm = sb("ktm", [128, NSUB, T], BF16)
        vaug = sb("vaug", [128, NSUB, H, 130], BF16)
        U = sb("U", [128, 5, T], BF16)
        poolT = sb("poolT", [128, 4, T], BF16)
        ybT = sb("ybT", [128, 4, T], BF16)
        spT = sb("spT", [128, 2, T], BF16)
        ytm = sb("ytm", [128, 2, T], BF16)
        yaT = sb("yaT", [128, H, T], BF16)
        Cf = sb("Cf", [128, H, 130], F32)
        Cbf = sb("Cbf", [128, H, 130], BF16)
        zif = sb("zif", [128, NSUB, 8], F32)
        ef = sb("ef", [128, NSUB, 4], F32)
        nlf = sb("nlf", [128, NSUB, 4], F32)
        eb = sb("eb", [128, NSUB, 4], F32)
        eB = sb("eB", [128, NSUB, 4], F32)
        inv2 = sb("inv2", [128, NSUB, 4], F32)
        tmpi = sb("tmpi", [128, NSUB, 4], F32)
        wgt = sb("wgt", [128, NSUB, 4], F32)
        sc = sb("sc", [128, 10, 4], F32)
        ss = sb("ss", [128, NSUB, 4], F32)
        frs = sb("frs", [128, 2, 4], F32)
        junk = sb("junk", [128, 128], BF16)
        dmy = sb("dmy", [128, 4], F32)
        cbf = sb("cbf", [128, NCST], BF16)
        c32 = sb("c32", [128, 384], F32)
        diag = sb("diag", [128, 32, 128], BF16)
        wsm = sb("wsm_s", [128, 576], BF16)
        pv = sb("pv_s", [128, NPV], F32)
        bif = sb("bif", [128, 8], F32)
        bv = sb("bv", [1, 512], BF16)
        gfin = sb("gfin", [128, D], F32)
        wslot = [sb(f"wslot{i}", [128, 4096], BF16) for i in range(NSLOT)]
        ps = es.enter_context(nc.psum_tensor("ps", [128, 4096], F32))
        psb = ps[:, :].bitcast(BF16)

        def PS(b, lo=0, hi=512):
            return ps[:, b * 512 + lo: b * 512 + hi]

        def PSB(b, lo, hi):
            return psb[:, b * 1024 + lo: b * 1024 + hi]

        ident32 = c32[:, C_ID:C_ID + 128]
        tri32 = c32[:, C_TRI:C_TRI + 128]
        ones32 = c32[:, C_ONE:C_ONE + 128]
        identb = cbf[:, C_ID:C_ID + 128]
        trib = cbf[:, C_TRI:C_TRI + 128]
        onesb = cbf[:, C_ONE:C_ONE + 128]

        def band(kind, g):
            o = C_BAND + (kind * 4 + g) * 128
            return cbf[:, o:o + 128]

        def pcol(c, n=1):
            return pv[:, c:c + n]

        def mm(out, lhsT, rhs, start, stop):
            P.op("pe", lambda e: e.matmul(out, lhsT, rhs, start=start, stop=stop),
                 reads=[lhsT, rhs], writes=[out])

        def tr(out, in_, ident):
            P.op("pe", lambda e: e.transpose(out, in_, ident), reads=[in_, ident], writes=[out])

        def act(out, in_, func, bias=None, scale=None, accum=None):
            kw = {}
            rd = [in_]
            wr = [out]
            if bias is not None:
                kw["bias"] = bias
                if not isinstance(bias, float):
                    rd.append(bias)
            if scale is not None:
                kw["scale"] = scale
                if not isinstance(scale, float):
                    rd.append(scale)
            if accum is not None:
                kw["accum_out"] = accum
                wr.append(accum)
            P.op("act", lambda e: e.activation(out, in_, func, **kw), reads=rd, writes=wr)

        def tt(out, in0, in1, op, eng="dve"):
            P.op(eng, lambda e: e.tensor_tensor(out, in0, in1, op), reads=[in0, in1], writes=[out])

        def stt(out, in0, scalar, in1, op0, op1):
            rd = [in0, in1] + ([] if isinstance(scalar, float) else [scalar])
            P.op("dve", lambda e: e.scalar_tensor_tensor(out, in0, scalar, in1, op0, op1),
                 reads=rd, writes=[out])

        def ts(out, in0, s1, op0, s2=None, op1=None):
            rd = [in0] + ([] if isinstance(s1, float) else [s1])
            if op1 is None:
                P.op("dve", lambda e: e.tensor_single_scalar(out, in0, s1, op0), reads=rd, writes=[out])
            else:
                P.op("dve", lambda e: e.tensor_scalar(out, in0, s1, s2, op0, op1), reads=rd, writes=[out])

        def cp(out, in_, eng="dve"):
            if eng == "act":
                P.op("act", lambda e: e.copy(out, in_), reads=[in_], writes=[out])
            else:
                P.op(eng, lambda e: e.tensor_copy(out, in_), reads=[in_], writes=[out])

        def memset(ap, v, eng="dve"):
            P.op(eng, lambda e: e.memset(ap, v), writes=[ap])

        def dma(eng, chan, out, in_, reads=(), writes=()):
            P.op(eng, lambda e: e.dma_start(out=out, in_=in_), reads=list(reads), writes=list(writes), chan=chan)

        def dump(name, ap, shape, dt=F32):
            if dbg is None or name not in dbg:
                return
            d = nc.dram_tensor("dbg_" + name, list(shape), dt, kind="ExternalOutput").ap()
            dbg_outs[name] = d
            dma("pool", P.dma_chan("dbg_" + name), d, ap, reads=[ap])

        ch_xin = P.dma_chan("xin")
        ch_out = [P.dma_chan(f"out{q}") for q in range(4)]
        ch_w = [P.dma_chan(f"w{i}") for i in range(NSLOT)]
        ch_st = [P.dma_chan(f"st{i}") for i in range(NSLOT)]
        ch_wc = [P.dma_chan(f"wc{i}") for i in range(NSLOT)]

        def load_x(i):
            src = x_d[i * T:(i + 1) * T, :].rearrange("(j p) d -> p j d", p=128)
            dma("pool", ch_xin, stage_in[:, :, :], src, writes=[stage_in[:, :, :]])

        for nm, dst, src in (
            ("pv", pv[:, :], pv_d),
            ("c32", c32[:, :], cst_d[:, 0:384]),
            ("cbf", cbf[:, :], cst_d),
            ("wsm", wsm[:, :], wsm_d),
            ("bif", bif[:, :], rowp_d[:, 0:8].partition_broadcast(128)),
            ("bv", bv[:, :], rowp_d[:, 8:520]),
            ("gfin", gfin[:, :], rowp_d[:, 520:1544].partition_broadcast(128)),
        ):
            dma("pool", P.dma_chan("su_" + nm), dst, src, writes=[dst])
        load_x(0)
        memset(dmy[:, :], 1.0)
        memset(Cf[:, :, :], 0.0)
        memset(Cbf[:, :, :], 0.0)
        memset(zqk[:, :, 0:3], 0.0)
        memset(U[:, 0, :], 0.0)
        memset(vaug[:, :, :, :], 0.0)
        for j in range(4):
            for ch in range(KC):
                ts(diag[:, j * 8 + ch, :], identb, pcol(CONV + j * 8 + ch), ALU.mult)

        wstate = {"n": 0}

        converted = set()

        def load_piece(k):
            s = wstate["n"] % NSLOT
            wstate["n"] += 1
            nco = 2816 if (11 <= k < 19 or 43 <= k < 51) else 4096
            if k not in converted:
                converted.add(k)
                dma("pool", ch_wc[s], wslot[s][:, 0:nco], wsrc_d[k][:, 0:nco], writes=[wslot[s][:, :]])
                dma("sp", ch_st[s], scr_d[k][:, 0:nco], wslot[s][:, 0:nco], reads=[wslot[s][:, :]], writes=[("scr", k, k + 1)])
            else:
                dma("sp", ch_w[s], wslot[s][:, 0:nco], scr_d[k][:, 0:nco], reads=[("scr", k, k + 1)], writes=[wslot[s][:, :]])
            return wslot[s]

        def preload_ln():
            act(dmy[:, 2:3], dmy[:, 0:1], AF.Ln)

        NBANK = 7

        sq_pending = []

        def sq_flush():
            while sq_pending:
                k, buf = sq_pending.pop(0)
                mm(PS(NBANK), onesb, buf[:, k, :], k == 0, k == KC - 1)

        def sq_chunk(k, buf):
            act(buf[:, k, :], xT[:, k, :], AF.Square)
            sq_flush()
            sq_pending.append((k, buf))

        def norm_finish(gcol):
            sq_flush()
            act(rs[:, 0, :], PS(NBANK), AF.Ln, bias=EPS, scale=1.0 / D)
            act(rs[:, 1, :], rs[:, 0, :], AF.Exp, scale=-0.5)
            for k in range(KC):
                stt(xn[:, k, :], xT[:, k, :], pcol(gcol + k), rs[:, 1, :], ALU.mult, ALU.mult)

        def ffn(piece0, gcol, after_chunk):
            norm_finish(gcol)
            w = load_piece(piece0)
            fb = [P.bank() for _ in range(4)]
            for k in range(KC):
                for gi in range(4):
                    col = k * 512 + (gi % 2) * 256 + (gi // 2) * 128
                    mm(PS(fb[gi]), w[:, col:col + 128], xn[:, k, :], k == 0, k == KC - 1)
            for jj in range(2):
                act(tmpf[:, jj % 2, :], PS(fb[2 * jj]), AF.Silu)
                tt(hid[:, jj, :], tmpf[:, jj % 2, :], PS(fb[2 * jj + 1]), ALU.mult)
            for p in range(1, 11):
                w = load_piece(piece0 + p)
                for jj in range(2):
                    j = 2 * p + jj
                    bg = P.bank()
                    for k in range(KC):
                        mm(PS(bg), w[:, k * 512 + jj * 128: k * 512 + jj * 128 + 128], xn[:, k, :], k == 0, k == KC - 1)
                    bu = P.bank()
                    for k in range(KC):
                        mm(PS(bu), w[:, k * 512 + 256 + jj * 128: k * 512 + 256 + jj * 128 + 128], xn[:, k, :], k == 0, k == KC - 1)
                    act(tmpf[:, j % 2, :], PS(bg), AF.Silu)
                    tt(hid[:, j, :], tmpf[:, j % 2, :], PS(bu), ALU.mult)
            preload_ln()
            for j in range(KC):
                w = load_piece(piece0 + 11 + j)
                b = P.bank()
                for k in range(NFC):
                    mm(PS(b), w[:, k * 128:(k + 1) * 128], hid[:, k, :], k == 0, k == NFC - 1)
                stt(xT[:, j, :], PS(b), 0.5, xT[:, j, :], ALU.mult, ALU.add)
                after_chunk(j)

        def proj_fm(w, cc, evac, bank=None):
            b = P.bank() if bank is None else bank
            for k in range(KC):
                mm(PS(b), w[:, k * 512 + cc * 128: k * 512 + cc * 128 + 128], xn[:, k, :], k == 0, k == KC - 1)
            evac(b)

        def mixer(i):
            norm_finish(GM)
            bgt = P.bank()
            for j in range(NSUB):
                for k in range(KC):
                    mm(PS(bgt, j * 8, j * 8 + 8), xn[:, k, j * 128:(j + 1) * 128], wsm[:, k * 8:(k + 1) * 8], k == 0, k == KC - 1)
            tt(zif[:, :, :], PS(bgt, 0, 32).rearrange("p (j c) -> p j c", j=NSUB),
               bif[:, :].unsqueeze(1).to_broadcast([128, NSUB, 8]), ALU.add)
            act(ef[:, :, :], zif[:, :, 4:8], AF.Exp, scale=-1.0)
            act(nlf[:, :, :], ef[:, :, :], AF.Ln, bias=1.0)
            bcs = P.bank()
            for j in range(NSUB):
                mm(PS(bcs, j * 4, j * 4 + 4), tri32, nlf[:, j, :], True, True)
            for j in range(NSUB):
                mm(PS(bcs, 16 + j * 4, 16 + j * 4 + 4), ones32, nlf[:, j, :], True, True)
            act(eb[:, :, :], PS(bcs, 0, 16).rearrange("p (j c) -> p j c", j=NSUB), AF.Exp, scale=-1.0)
            act(eB[:, :, :], PS(bcs, 16, 32).rearrange("p (j c) -> p j c", j=NSUB), AF.Exp, scale=-1.0)
            act(inv2[:, :, :], PS(bcs, 0, 16).rearrange("p (j c) -> p j c", j=NSUB), AF.Exp, scale=2.0)
            tt(tmpi[:, :, :], zif[:, :, 0:4], PS(bcs, 0, 16).rearrange("p (j c) -> p j c", j=NSUB), ALU.add)
            act(wgt[:, :, :], tmpi[:, :, :], AF.Exp)
            cp(vaug[:, :, :, 128:129], wgt[:, :, :].unsqueeze(3))
            if stop == "mx_gates":
                return True
            for pc in range(2):
                w = load_piece(19 + pc)
                for cc in range(4):
                    ch = pc * 4 + cc
                    proj_fm(w, cc, lambda b, ch=ch: ts(zqk[:, ch, 3:515], PS(b), pcol(BQK + ch), ALU.add))
            if stop == "mx_qk":
                return True
            for ch in range(KC):
                b = P.bank()
                for j in range(4):
                    mm(PS(b), diag[:, j * 8 + ch, :], zqk[:, ch, j:j + 512], j == 0, j == 3)
                act(qkT[:, ch, :], PS(b), AF.Silu)
            cp(zqk[:, :, 0:3], zqk[:, :, 512:515])
            if stop == "mx_conv":
                return True
            for j in range(NSUB):
                b = P.bank()
                for h in range(H):
                    tr(PSB(b, h * 128, (h + 1) * 128), qkT[:, 4 + h, j * 128:(j + 1) * 128], identb)
                cp(ktm[:, j, :], PSB(b, 0, 512), eng="act")
            if stop == "mx_ktr":
                return True
            w = load_piece(21)
            for j in range(NSUB):
                b = P.bank()
                for k in range(KC):
                    mm(PS(b), xn[:, k, j * 128:(j + 1) * 128], w[:, k * 512:(k + 1) * 512], k == 0, False)
                mm(PS(b), cbf[0:1, C_ONE:C_ONE + 128], bv[0:1, :], False, True)
                tt(vaug[:, j, :, 0:128], PS(b).rearrange("p (h v) -> p h v", h=H),
                   wgt[:, j, :].unsqueeze(2).to_broadcast([128, H, 128]), ALU.mult)
            if stop == "mx_v":
                return True
            w = load_piece(22)
            for cc in range(4):
                proj_fm(w, cc, lambda b, cc=cc: act(hid[:, 16 + cc, :], PS(b), AF.Sigmoid, bias=pcol(BO + cc)))
            if stop == "mx_o":
                return True
            preload_ln()
            if i > 0:
                cp(U[:, 0, :], U[:, 4, :])
            w = load_piece(23)
            for j in range(NSUB):
                b = P.bank()
                for k in range(KC):
                    mm(PS(b), xn[:, k, j * 128:(j + 1) * 128], w[:, k * 512:(k + 1) * 512], k == 0, k == KC - 1)
                cp(U[:, 1 + j, :], PS(b), eng="act")
            for g in range(4):
                b = P.bank()
                for j in range(NSUB):
                    first = first_tile_global and i == 0 and j == 0
                    mm(PS(b, j * 128, (j + 1) * 128), U[:, j, g * 128:(g + 1) * 128], band(1, g), True, False)
                    mm(PS(b, j * 128, (j + 1) * 128), U[:, j + 1, g * 128:(g + 1) * 128], band(2 if first else 0, g), False, True)
                cp(poolT[:, g, :], PS(b))
            for g in range(4):
                b = P.bank()
                mm(PS(b), wsm[:, 64 + g * 128: 64 + (g + 1) * 128], poolT[:, g, :], True, True)
                act(ybT[:, g, :], PS(b), AF.Copy, scale=pcol(PSC + g))
            if stop == "mx_pool":
                return True
            gate_w = {}

            def gate_filler(which, c):
                def run(bank=None):
                    pidx = (24 if which == 0 else 26) + c // 4
                    if pidx not in gate_w:
                        gate_w[pidx] = load_piece(pidx)
                    bcol = (BGA if which == 0 else BGB) + c
                    dst = hid[:, (0 if which == 0 else 8) + c, :]
                    proj_fm(gate_w[pidx], c % 4, lambda b: act(dst, PS(b), AF.Identity, bias=pcol(bcol)), bank=bank)
                return run

            fillers = [gate_filler(wh, c) for wh in range(2) for c in range(KC)]

            def fill():
                if fillers:
                    fillers.pop(0)()

            memset(ss[:, :, :], 0.0)
            small = {"n": 0}

            def small_bank():
                small["n"] += 1
                return 6 + small["n"] % 2

            ACCB = (0, 2)
            CBANK = 4
            S_ = lambda n: sc[:, n, :]

            def st_S(j):
                tok = slice(j * 128, (j + 1) * 128)
                bS = small_bank()
                for h in range(H):
                    mm(PS(bS, h * 128, (h + 1) * 128), qkT[:, 4 + h, tok], qkT[:, h, tok], True, True)
                tt(spT[:, j % 2, :].rearrange("p (h t) -> p h t", h=H), PS(bS).rearrange("p (h t) -> p h t", h=H),
                   trib.unsqueeze(1).to_broadcast([128, H, 128]), ALU.mult)

            def st_A(j):
                tok = slice(j * 128, (j + 1) * 128)
                par = j % 2
                bA = ACCB[par]
                for h in range(H):
                    o_ = ps[:, bA * 512 + h * 256: bA * 512 + h * 256 + 130]
                    mm(o_, spT[:, par, h * 128:(h + 1) * 128], vaug[:, j, h, 0:130], True, False)
                    mm(o_, qkT[:, h, tok], Cbf[:, h, 0:130], False, True)
                acc = ps[:, bA * 512: bA * 512 + 1024].rearrange("p (h c) -> p h c", h=H)
                for h in range(H):
                    act(junk[:, :], acc[:, h, 0:128], AF.Square, accum=ss[:, j, h:h + 1])
                bC = CBANK
                for h in range(H):
                    mm(ps[:, bC * 512 + h * 256: bC * 512 + h * 256 + 130], ktm[:, j, h * 128:(h + 1) * 128], vaug[:, j, h, 0:130], True, True)
                accC = ps[:, bC * 512: bC * 512 + 1024].rearrange("p (h c) -> p h c", h=H)
                tt(Cf[:, :, :], Cf[:, :, :], accC[:, :, 0:130], ALU.add)
                tt(Cf[:, :, :], Cf[:, :, :], eB[:, j, :].unsqueeze(2).to_broadcast([128, H, 130]), ALU.mult)
                cp(Cbf[:, :, :], Cf[:, :, :])

            def st_scal(j):
                par = j % 2
                bA = ACCB[par]
                acc = ps[:, bA * 512: bA * 512 + 1024].rearrange("p (h c) -> p h c", h=H)
                stt(S_(0).unsqueeze(2), acc[:, :, 128:129], SCALE, eb[:, j, :].unsqueeze(2), ALU.mult, ALU.mult)
                tt(S_(1), S_(0), S_(0), ALU.mult)
                ts(S_(2), S_(1), 1.0, ALU.max, EPS / (SCALE * SCALE), ALU.mult)
                tt(S_(3), S_(2), inv2[:, j, :], ALU.mult)
                stt(S_(4), ss[:, j, :], 1.0 / 128.0, S_(3), ALU.mult, ALU.add)
                act(S_(5), S_(4), AF.Ln)
                act(S_(6), S_(5), AF.Exp, scale=-0.5)
                tt(ytm[:, par, :].rearrange("p (h v) -> p h v", h=H), acc[:, :, 0:128],
                   S_(6).unsqueeze(2).to_broadcast([128, H, 128]), ALU.mult)

            def st_T(j):
                tok = slice(j * 128, (j + 1) * 128)
                par = j % 2
                bT = small_bank()
                for h in range(H):
                    tr(PSB(bT, h * 128, (h + 1) * 128), ytm[:, par, h * 128:(h + 1) * 128], identb)
                for h in range(H):
                    stt(yaT[:, h, tok], PSB(bT, h * 128, (h + 1) * 128), pcol(HNG + h), hid[:, 16 + h, tok], ALU.mult, ALU.mult)

            def fillS():
                if fillers:
                    fillers.pop(0)(small_bank())

            st_S(0)
            fillS()
            st_A(0)
            fillS()
            for j in range(NSUB):
                if j + 1 < NSUB:
                    st_S(j + 1)
                    fillS()
                st_scal(j)
                if j + 1 < NSUB:
                    st_A(j + 1)
                    fillS()
                if j >= 1:
                    st_T(j - 1)
                    fillS()
            st_T(NSUB - 1)
            if stop == "mx_lstm":
                return True
            while fillers:
                fillers.pop(0)()
            for c in range(KC):
                act(hid[:, c, :], hid[:, c, :], AF.Sigmoid)
                act(hid[:, 8 + c, :], hid[:, 8 + c, :], AF.Sigmoid)
            preload_ln()
            wa = load_piece(28)
            wb = load_piece(29)
            for c in range(KC):
                ba = P.bank()
                for k in range(4):
                    mm(PS(ba), wa[:, k * 1024 + c * 128: k * 1024 + c * 128 + 128], yaT[:, k, :], k == 0, k == 3)
                bb = P.bank()
                for k in range(4):
                    mm(PS(bb), wb[:, k * 1024 + c * 128: k * 1024 + c * 128 + 128], ybT[:, k, :], k == 0, k == 3)
                tt(tmpb[:, 0, :], PS(ba), hid[:, c, :], ALU.mult)
                tt(tmpb[:, 1, :], PS(bb), hid[:, 8 + c, :], ALU.mult)
                tt(xn[:, c, :], tmpb[:, 0, :], tmpb[:, 1, :], ALU.add)
            if stop == "mx_merge":
                return True
            for pc in range(2):
                w = load_piece(30 + pc)
                for cc in range(4):
                    c = pc * 4 + cc
                    proj_fm(w, cc, lambda b, c=c: tt(xT[:, c, :], PS(b), xT[:, c, :], ALU.add))
                    sq_chunk(c, qkT)

        for i in range(nt if stop != "setup" else 0):
            if i > 0:
                P.new_epoch()
            for k in range(KC):
                b = (6, 2, 3, 4, 5, 6, 2, 3)[k]
                for j in range(NSUB):
                    tr(PS(b, j * 128, (j + 1) * 128), stage_in[:, j, k * 128:(k + 1) * 128], ident32)
                cp(xT[:, k, :], PS(b), eng=("act" if (k < 4 or k % 2) else "dve"))
                sq_chunk(k, xn)
            if i + 1 < nt:
                load_x(i + 1)
            if i == 0:
                dump("x0", xT[:, :, :], [128, KC, T])
            if stop == "input":
                break
            ffn(0, G1, lambda j: sq_chunk(j, xn))
            if i == 0:
                dump("x1", xT[:, :, :], [128, KC, T])
            if stop == "ffn1":
                break
            if mixer(i):
                break
            if i == 0:
                dump("x2", xT[:, :, :], [128, KC, T])
                dump("qkT", qkT[:, :, :], [128, KC, T], BF16)
                dump("yaT", yaT[:, :, :], [128, H, T], BF16)
                dump("ybT", ybT[:, :, :], [128, 4, T], BF16)
                dump("vaug", vaug[:, :, :, :], [128, NSUB, H, 130], BF16)
                dump("eb", eb[:, :, :], [128, NSUB, 4])
                dump("wgt", wgt[:, :, :], [128, NSUB, 4])
                dump("Cf", Cf[:, :, :], [128, H, 130])
            if stop == "mixer":
                break
            ffn(32, G2, lambda j: act(xn[:, j, :], xT[:, j, :], AF.Square))
            if i == 0:
                dump("x3", xT[:, :, :], [128, KC, T])
            if stop == "ffn2":
                break
            bf = 6
            for j in range(NSUB):
                for k in range(KC):
                    mm(PS(bf, j, j + 1), xn[:, k, j * 128:(j + 1) * 128], onesb[:, 0:1], k == 0, k == KC - 1)
            act(frs[:, 0, :], PS(bf, 0, 4), AF.Ln, bias=EPS, scale=1.0 / D)
            act(frs[:, 1, :], frs[:, 0, :], AF.Exp, scale=-0.5)
            for j in range(NSUB):
                b2 = (0, 2, 4, 0)[j]
                for k in range(KC):
                    tr(ps[:, b2 * 512 + k * 128: b2 * 512 + (k + 1) * 128], xT[:, k, j * 128:(j + 1) * 128], ident32)
                so = stage_out[:, j, :]
                stt(so, ps[:, b2 * 512: b2 * 512 + 1024], frs[:, 1, j:j + 1], gfin[:, :], ALU.mult, ALU.mult)
                r0 = i * T + j * 128
                dma("pool", ch_out[j], out_d[r0:r0 + 128, :], so, reads=[so])

        P.finalize(nc, es)
    return nc, dbg_outs


def _kmaj(W, c0, n):
    K = W.shape[0]
    return W[:, c0:c0 + n].reshape(K // 128, 128, n).transpose(1, 0, 2)


def _pad(a):
    a = a.reshape(128, -1)
    out = np.zeros((128, 4096), np.float32)
    out[:, :a.shape[1]] = a
    return out


def _consts():
    c = np.zeros((128, NCST), np.float32)
    s = np.arange(128)[:, None]
    t = np.arange(128)[None, :]
    c[:, C_ID:C_ID + 128] = (s == t)
    c[:, C_TRI:C_TRI + 128] = (s <= t)
    c[:, C_ONE:C_ONE + 128] = 1.0
    for g, w in enumerate((2, 4, 8, 16)):
        inwin = ((t - s) >= 0) & ((t - s) <= w - 1)
        cur = inwin / w - (s == t)
        prev = ((t - s + 128) <= w - 1) / w
        cnt = np.minimum(t + 1, w)
        first = inwin / cnt - (s == t)
        c[:, C_BAND + (0 * 4 + g) * 128: C_BAND + (0 * 4 + g + 1) * 128] = cur
        c[:, C_BAND + (1 * 4 + g) * 128: C_BAND + (1 * 4 + g + 1) * 128] = prev
        c[:, C_BAND + (2 * 4 + g) * 128: C_BAND + (2 * 4 + g + 1) * 128] = first
    return c


def _layout(inp):
    f = lambda a: np.asarray(a, dtype=np.float32)
    pieces = np.zeros((NPIECE, 128, 4096), np.float32)

    def ffn_pieces(base, wgu, wd):
        for p in range(11):
            a = np.concatenate([_kmaj(wgu, 256 * p, 256), _kmaj(wgu, DFF + 256 * p, 256)], axis=2)
            pieces[base + p] = a.reshape(128, 4096)
        for j in range(8):
            pieces[base + 11 + j] = _pad(_kmaj(wd, 128 * j, 128))

    ffn_pieces(0, f(inp["ffn1_w_gu"])[0], f(inp["ffn1_w_down"])[0])
    ffn_pieces(32, f(inp["ffn2_w_gu"])[0], f(inp["ffn2_w_down"])[0])
    win = f(inp["w_in"])[0]
    for n, c0 in ((19, 0), (20, 512), (21, 1024), (22, 1536), (23, 2048), (24, 2560), (25, 3072), (26, 3584), (27, 4096)):
        pieces[n] = _kmaj(win, c0, 512).reshape(128, 4096)
    pieces[28] = _kmaj(f(inp["p_a"])[0], 0, 1024).reshape(128, 4096)
    pieces[29] = _kmaj(f(inp["p_b"])[0], 0, 1024).reshape(128, 4096)
    wo = f(inp["w_out"])[0]
    pieces[30] = _kmaj(wo, 0, 512).reshape(128, 4096)
    pieces[31] = _kmaj(wo, 512, 512).reshape(128, 4096)

    col = lambda v: f(v).reshape(-1, 128).T
    pvv = np.zeros((128, NPV), np.float32)
    b_in = f(inp["b_in"])[0]
    pvv[:, G1:G1 + 8] = col(inp["ffn1_norm_g"][0])
    pvv[:, GM:GM + 8] = col(inp["mix_norm_g"][0])
    pvv[:, G2:G2 + 8] = col(inp["ffn2_norm_g"][0])
    pvv[:, BQK:BQK + 8] = col(b_in[0:1024])
    pvv[:, BO:BO + 4] = col(b_in[1536:2048])
    pvv[:, BGA:BGA + 8] = col(b_in[2560:3584])
    pvv[:, BGB:BGB + 8] = col(b_in[3584:4608])
    pvv[:, HNG:HNG + 4] = col(inp["head_norm_g"][0])
    pvv[:, PSC:PSC + 4] = col(inp["pool_scale"][0])
    cw = f(inp["conv_qk"])[0]
    for j in range(4):
        pvv[:, CONV + j * 8: CONV + j * 8 + 8] = col(cw[j])
    rowp = np.concatenate([b_in[4608:4616], b_in[1024:1536], f(inp["final_norm_g"])]).reshape(1, -1)
    wsmv = np.zeros((128, 576), np.float32)
    wsmv[:, 0:64] = _kmaj(win, 4608, 8).reshape(128, 64)
    wsmv[:, 64:576] = f(inp["w_pool"])[0].transpose(1, 0, 2).reshape(128, 512)
    return {"wsrc": pieces, "pv": pvv, "rowp": np.ascontiguousarray(rowp), "wsm": wsmv, "cst": _consts()}


_NC_CACHE = {}


def kernel(**inputs):
    x = np.asarray(inputs["x"], dtype=np.float32)
    shared = _layout(inputs)
    nt = SEQ // T
    if nt not in _NC_CACHE:
        _NC_CACHE[nt] = build_nc(nt)[0]
    nc = _NC_CACHE[nt]
    in_maps = [dict(shared, x=np.ascontiguousarray(x[b])) for b in range(NB)]
    res = run_bass_kernel_spmd(nc, in_maps, core_ids=list(range(NB)))
    return np.stack([np.asarray(r["out"]) for r in res.results], axis=0).astype(np.float32)
```
